# Optimizing a Trainium2 kernel written in Bass

```python
import math
import jax, jax.numpy as jnp
from jax import lax
import numpy as np

D_MODEL = 1024
BATCH = 16
SEQ = 2048
DEPTH = 2

MEM_LEN = 256
GDN_HEADS = 4
GDN_DK = 128
GDN_DV = 128
GDN_QK = GDN_HEADS * GDN_DK
GDN_V = GDN_HEADS * GDN_DV
GDN_CONV = 4
GDN_CHUNK = 64
FOX_HEADS = 8
FOX_DH = 64
FOX_W = FOX_HEADS * FOX_DH
FOX_BLOCK = 128
GLA_HEADS = 4
GLA_DK = 64
GLA_DV = 128
GLA_QK = GLA_HEADS * GLA_DK
GLA_V = GLA_HEADS * GLA_DV
GLA_RANK = 16
GLA_TAU = 16.0
GLA_CHUNK = 64
N_BRANCH = 3
XA_HEADS = 4
XA_DH = D_MODEL // XA_HEADS
MOE_GROUPS = 4
MOE_PER_GROUP = 8
MOE_EXPERTS = MOE_GROUPS * MOE_PER_GROUP
MOE_TOPK = 2
MOE_FF = D_MODEL // 4
MOE_BLOCK = 256
DEEPNORM_ALPHA = (2 * DEPTH) ** 0.25
DEEPNORM_BETA = (8 * DEPTH) ** -0.25
LN_EPS = 1e-5
RMS_EPS = 1e-6
IN_SPLITS = (2 * GDN_QK + GDN_V, GDN_HEADS, GDN_HEADS, GDN_V,
             3 * FOX_W, FOX_HEADS,
             GLA_QK, GLA_QK, GLA_V, GLA_V, GLA_RANK,
             N_BRANCH * D_MODEL)
N_IN = sum(IN_SPLITS)

kernel_name = 'hybrid_gdn_fox_gla_hmoe_block'

F32 = jnp.float32


def layer_norm(x, g, b):
    xf = x.astype(F32)
    mu = jnp.mean(xf, axis=-1, keepdims=True)
    var = jnp.mean(jnp.square(xf - mu), axis=-1, keepdims=True)
    return ((xf - mu) * lax.rsqrt(var + LN_EPS) * g.astype(F32) + b.astype(F32)).astype(x.dtype)


def rms_norm(x, w):
    xf = x.astype(F32)
    return (xf * lax.rsqrt(jnp.mean(xf * xf, axis=-1, keepdims=True) + RMS_EPS) * w.astype(F32)).astype(x.dtype)


def l2_normalize(x):
    xf = x.astype(F32)
    return (xf * lax.rsqrt(jnp.sum(xf * xf, axis=-1, keepdims=True) + RMS_EPS)).astype(x.dtype)


def causal_depthwise_conv(x, w):
    width, s = w.shape[0], x.shape[1]
    xp = jnp.pad(x, ((0, 0), (width - 1, 0), (0, 0)))
    return sum(xp[:, i:i + s, :] * w[i] for i in range(width))


def _to_chunks(t, n, c):
    b, s, h = t.shape[:3]
    t = t.astype(F32).reshape((b, n, c, h) + t.shape[3:])
    return jnp.moveaxis(t, 3, 1)


def gated_delta_rule_chunked(q, k, v, g, beta):
    out_dtype = v.dtype
    bsz, s, h, dk = q.shape
    dv = v.shape[-1]
    c = GDN_CHUNK
    n = s // c
    q = _to_chunks(q, n, c) * (dk ** -0.5)
    k = _to_chunks(k, n, c)
    v = _to_chunks(v, n, c)
    g = jnp.cumsum(_to_chunks(g, n, c), axis=-1)
    beta = _to_chunks(beta, n, c)
    causal = jnp.tril(jnp.ones((c, c), bool))
    strict = jnp.tril(jnp.ones((c, c), bool), -1)
    diff = g[..., :, None] - g[..., None, :]
    decay = jnp.where(causal, jnp.exp(jnp.where(causal, diff, 0.0)), 0.0)
    k_beta = k * beta[..., None]
    lower = jnp.where(strict, jnp.einsum('bhnid,bhnjd->bhnij', k_beta, k) * decay, 0.0)
    eye = jnp.eye(c, dtype=F32)
    t_inv = lax.linalg.triangular_solve(eye + lower, jnp.broadcast_to(eye, lower.shape),
                                        left_side=True, lower=True, unit_diagonal=True)
    u = jnp.einsum('bhnij,bhnje->bhnie', t_inv, v * beta[..., None])
    w = jnp.einsum('bhnij,bhnjd->bhnid', t_inv, k_beta * jnp.exp(g)[..., None])
    attn = jnp.where(causal, jnp.einsum('bhnid,bhnjd->bhnij', q, k) * decay, 0.0)
    q_dec = q * jnp.exp(g)[..., None]
    k_dec = k * jnp.exp(g[..., -1:] - g)[..., None]
    chunk_decay = jnp.exp(g[..., -1])

    def step(state, xs):
        attn_c, u_c, w_c, qd_c, kd_c, cd_c = xs
        v_new = u_c - jnp.einsum('bhcd,bhde->bhce', w_c, state)
        out = jnp.einsum('bhcd,bhde->bhce', qd_c, state) + jnp.einsum('bhij,bhje->bhie', attn_c, v_new)
        state = state * cd_c[..., None, None] + jnp.einsum('bhcd,bhce->bhde', kd_c, v_new)
        return state, out

    xs = tuple(jnp.moveaxis(t, 2, 0) for t in (attn, u, w, q_dec, k_dec, chunk_decay))
    _, out = lax.scan(step, jnp.zeros((bsz, h, dk, dv), F32), xs)
    return jnp.transpose(out, (1, 0, 3, 2, 4)).reshape(bsz, s, h, dv).astype(out_dtype)


def gla_chunked(q, k, v, log_a):
    out_dtype = v.dtype
    bsz, s, h, dk = q.shape
    dv = v.shape[-1]
    c = GLA_CHUNK
    n = s // c
    q = _to_chunks(q, n, c) * (dk ** -0.5)
    k = _to_chunks(k, n, c)
    v = _to_chunks(v, n, c)
    cum = jnp.cumsum(_to_chunks(log_a, n, c), axis=3)
    q_t = q * jnp.exp(cum)
    k_t = k * jnp.exp(-cum)
    causal = jnp.tril(jnp.ones((c, c), bool))
    attn = jnp.where(causal, jnp.einsum('bhnid,bhnjd->bhnij', q_t, k_t), 0.0)
    intra = jnp.einsum('bhnij,bhnje->bhnie', attn, v)
    k_dec = k * jnp.exp(cum[..., -1:, :] - cum)
    chunk_decay = jnp.exp(cum[..., -1, :])

    def step(state, xs):
        qt_c, kd_c, v_c, cd_c = xs
        out = jnp.einsum('bhcd,bhde->bhce', qt_c, state)
        state = state * cd_c[..., None] + jnp.einsum('bhcd,bhce->bhde', kd_c, v_c)
        return state, out

    xs = tuple(jnp.moveaxis(t, 2, 0) for t in (q_t, k_dec, v, chunk_decay))
    _, inter = lax.scan(step, jnp.zeros((bsz, h, dk, dv), F32), xs)
    out = intra + jnp.moveaxis(inter, 0, 2)
    return jnp.transpose(out, (0, 2, 3, 1, 4)).reshape(bsz, s, h, dv).astype(out_dtype)


def forgetting_attention(q, k, v, log_f):
    bsz, s, h, dh = q.shape
    nb = s // FOX_BLOCK
    cum_k = jnp.transpose(jnp.cumsum(log_f.astype(F32), axis=1), (0, 2, 1))
    kf = k.astype(F32)
    q_blocks = jnp.moveaxis(q.astype(F32).reshape(bsz, nb, FOX_BLOCK, h, dh), 1, 0)
    c_blocks = jnp.moveaxis(cum_k.reshape(bsz, h, nb, FOX_BLOCK), 2, 0)
    key_pos = jnp.arange(s)

    def block(args):
        q_blk, c_blk, start = args
        logits = (jnp.einsum('bqhd,bkhd->bhqk', q_blk, kf) * (dh ** -0.5)
                  + c_blk[..., :, None] - cum_k[:, :, None, :])
        query_pos = start + jnp.arange(FOX_BLOCK)
        logits = jnp.where(key_pos[None, :] <= query_pos[:, None], logits, -jnp.inf)
        p = jax.nn.softmax(logits, axis=-1)
        return jnp.einsum('bhqk,bkhd->bqhd', p.astype(v.dtype), v)

    out = lax.map(block, (q_blocks, c_blocks, jnp.arange(nb) * FOX_BLOCK))
    return jnp.moveaxis(out, 0, 1).reshape(bsz, s, h, dh)


def hybrid_token_mixer(x, w_in, gdn_conv_w, gdn_a_log, gdn_dt_bias, gdn_norm_w, fox_f_bias,
                       gla_w_gate2, gla_b_gate, gla_norm_w, p_gdn, p_fox, p_gla, b_merge, w_out):
    bsz, s, d = x.shape
    proj = x @ w_in
    offsets = np.cumsum(IN_SPLITS)[:-1].tolist()
    (gdn_qkv, gdn_b, gdn_a, gdn_z, fox_qkv, fox_f,
     gla_q, gla_k, gla_v, gla_r, gla_lr, merge_logits) = jnp.split(proj, offsets, axis=-1)

    gdn_qkv = jax.nn.silu(causal_depthwise_conv(gdn_qkv, gdn_conv_w))
    q_a, k_a, v_a = jnp.split(gdn_qkv, [GDN_QK, 2 * GDN_QK], axis=-1)
    q_a = l2_normalize(q_a.reshape(bsz, s, GDN_HEADS, GDN_DK))
    k_a = l2_normalize(k_a.reshape(bsz, s, GDN_HEADS, GDN_DK))
    v_a = v_a.reshape(bsz, s, GDN_HEADS, GDN_DV)
    beta_a = jax.nn.sigmoid(gdn_b)
    g_a = -jnp.exp(gdn_a_log) * jax.nn.softplus(gdn_a + gdn_dt_bias)
    o_a = gated_delta_rule_chunked(q_a, k_a, v_a, g_a, beta_a)
    o_a = rms_norm(o_a, gdn_norm_w) * jax.nn.silu(gdn_z.reshape(bsz, s, GDN_HEADS, GDN_DV))

    q_b, k_b, v_b = jnp.split(fox_qkv, 3, axis=-1)
    log_f = jax.nn.log_sigmoid((fox_f + fox_f_bias).astype(F32))
    o_b = forgetting_attention(q_b.reshape(bsz, s, FOX_HEADS, FOX_DH),
                               k_b.reshape(bsz, s, FOX_HEADS, FOX_DH),
                               v_b.reshape(bsz, s, FOX_HEADS, FOX_DH), log_f)

    log_a = jax.nn.log_sigmoid((gla_lr @ gla_w_gate2 + gla_b_gate).astype(F32)) / GLA_TAU
    o_c = gla_chunked(gla_q.reshape(bsz, s, GLA_HEADS, GLA_DK),
                      gla_k.reshape(bsz, s, GLA_HEADS, GLA_DK),
                      gla_v.reshape(bsz, s, GLA_HEADS, GLA_DV),
                      log_a.reshape(bsz, s, GLA_HEADS, GLA_DK))
    o_c = rms_norm(o_c, gla_norm_w) * jax.nn.silu(gla_r.reshape(bsz, s, GLA_HEADS, GLA_DV))

    gate = jax.nn.sigmoid(merge_logits + b_merge).reshape(bsz, s, N_BRANCH, d)
    merged = (gate[:, :, 0] * (o_a.reshape(bsz, s, GDN_V) @ p_gdn)
              + gate[:, :, 1] * (o_b.reshape(bsz, s, FOX_W) @ p_fox)
              + gate[:, :, 2] * (o_c.reshape(bsz, s, GLA_V) @ p_gla))
    return (merged @ w_out).astype(x.dtype)


def memory_cross_attention(x, mem, wq, wkv, wo):
    bsz, s, d = x.shape
    m = mem.shape[1]
    q = (x @ wq).reshape(bsz, s, XA_HEADS, XA_DH)
    k, v = jnp.split(mem @ wkv, 2, axis=-1)
    k = k.reshape(bsz, m, XA_HEADS, XA_DH)
    v = v.reshape(bsz, m, XA_HEADS, XA_DH)
    logits = jnp.einsum('bqhd,bkhd->bhqk', q.astype(F32), k.astype(F32)) * (XA_DH ** -0.5)
    p = jax.nn.softmax(logits, axis=-1).astype(v.dtype)
    o = jnp.einsum('bhqk,bkhd->bqhd', p, v).reshape(bsz, s, d)
    return (o @ wo).astype(x.dtype)


def routed_experts(xf, expert_idx, expert_w, w_gate, w_up, w_down):
    n, d = xf.shape
    kk = expert_idx.shape[1]
    e = w_gate.shape[0]
    nk = n * kk
    flat_e = expert_idx.reshape(nk)
    order = jnp.argsort(flat_e)
    sorted_e = flat_e[order]
    sorted_tok = order // kk
    counts = jnp.zeros((e,), jnp.int32).at[flat_e].add(1)
    padded = (counts + MOE_BLOCK - 1) // MOE_BLOCK * MOE_BLOCK
    pad_end = jnp.cumsum(padded)
    pad_start = pad_end - padded
    start = jnp.cumsum(counts) - counts
    dest = pad_start[sorted_e] + (jnp.arange(nk, dtype=jnp.int32) - start[sorted_e])
    total = (nk + MOE_BLOCK - 1) // MOE_BLOCK * MOE_BLOCK + e * MOE_BLOCK
    n_blocks = total // MOE_BLOCK
    slot_tok = jnp.full((total,), n, jnp.int32).at[dest].set(sorted_tok)
    blk_expert = jnp.minimum(
        jnp.searchsorted(pad_end, jnp.arange(n_blocks, dtype=jnp.int32) * MOE_BLOCK, side='right'), e - 1)
    x_pad = jnp.concatenate([xf, jnp.zeros((1, d), xf.dtype)], axis=0)
    xb = x_pad[slot_tok].reshape(n_blocks, MOE_BLOCK, d)

    def one_block(args):
        x_blk, ex = args
        hidden = jax.nn.silu(x_blk @ w_gate[ex]) * (x_blk @ w_up[ex])
        return hidden @ w_down[ex]

    yb = lax.map(one_block, (xb, blk_expert)).reshape(total, d)
    slot_of_assign = jnp.zeros((nk,), jnp.int32).at[order].set(dest)
    y = yb[slot_of_assign].reshape(n, kk, d)
    return jnp.einsum('nk,nkd->nd', expert_w, y)


def hierarchical_moe(x, w_group, b_group, w_expert, b_expert, w_gate, w_up, w_down):
    bsz, s, d = x.shape
    xf = x.reshape(bsz * s, d)
    p_group = jax.nn.softmax((xf @ w_group + b_group).astype(F32), axis=-1)
    p_top, g_sel = lax.top_k(p_group, 1)
    e_logits = (xf @ w_expert + b_expert).astype(F32).reshape(-1, MOE_GROUPS, MOE_PER_GROUP)
    sel_logits = jnp.take_along_axis(e_logits, g_sel[:, :, None], axis=1)[:, 0]
    p_in = jax.nn.softmax(sel_logits, axis=-1)
    w_top, e_local = lax.top_k(p_in, MOE_TOPK)
    w_top = w_top / jnp.sum(w_top, axis=-1, keepdims=True) * p_top
    e_global = g_sel * MOE_PER_GROUP + e_local
    y = routed_experts(xf, e_global, w_top.astype(x.dtype), w_gate, w_up, w_down)
    return y.reshape(bsz, s, d).astype(x.dtype)


def setup_inputs(seed: int = 0) -> dict:
    key = jax.random.key(seed)
    ks = iter(jax.random.split(key, 40))
    L, D = DEPTH, D_MODEL

    def normal(shape, scale):
        return jax.random.normal(next(ks), shape, F32) * scale

    def uniform(shape, lo, hi):
        return jax.random.uniform(next(ks), shape, F32, minval=lo, maxval=hi)

    dt = jnp.exp(uniform((L, GDN_HEADS), math.log(1e-3), math.log(0.1)))
    return {
        'x': normal((BATCH, SEQ, D), 1.0),
        'mem': normal((BATCH, MEM_LEN, D), 1.0),
        'w_in': normal((L, D, N_IN), D ** -0.5),
        'gdn_conv_w': normal((L, GDN_CONV, 2 * GDN_QK + GDN_V), GDN_CONV ** -0.5),
        'gdn_a_log': jnp.log(uniform((L, GDN_HEADS), 1.0, 16.0)),
        'gdn_dt_bias': dt + jnp.log(-jnp.expm1(-dt)),
        'gdn_norm_w': 1.0 + normal((L, GDN_DV), 0.02),
        'fox_f_bias': uniform((L, FOX_HEADS), 2.0, 5.0),
        'gla_w_gate2': normal((L, GLA_RANK, GLA_QK), GLA_RANK ** -0.5),
        'gla_b_gate': normal((L, GLA_QK), 0.1),
        'gla_norm_w': 1.0 + normal((L, GLA_DV), 0.02),
        'p_gdn': normal((L, GDN_V, D), GDN_V ** -0.5),
        'p_fox': normal((L, FOX_W, D), FOX_W ** -0.5),
        'p_gla': normal((L, GLA_V, D), GLA_V ** -0.5),
        'b_merge': normal((L, N_BRANCH * D), 0.1),
        'w_out': normal((L, D, D), DEEPNORM_BETA * D ** -0.5),
        'ln1_g': 1.0 + normal((L, D), 0.02),
        'ln1_b': normal((L, D), 0.02),
        'xa_wq': normal((L, D, D), D ** -0.5),
        'xa_wkv': normal((L, D, 2 * D), D ** -0.5),
        'xa_wo': normal((L, D, D), DEEPNORM_BETA * D ** -0.5),
        'ln2_g': 1.0 + normal((L, D), 0.02),
        'ln2_b': normal((L, D), 0.02),
        'moe_w_group': normal((L, D, MOE_GROUPS), D ** -0.5),
        'moe_b_group': normal((L, MOE_GROUPS), 0.01),
        'moe_w_expert': normal((L, D, MOE_EXPERTS), D ** -0.5),
        'moe_b_expert': normal((L, MOE_EXPERTS), 0.01),
        'moe_w_gate': normal((L, MOE_EXPERTS, D, MOE_FF), D ** -0.5),
        'moe_w_up': normal((L, MOE_EXPERTS, D, MOE_FF), D ** -0.5),
        'moe_w_down': normal((L, MOE_EXPERTS, MOE_FF, D), DEEPNORM_BETA * MOE_FF ** -0.5),
        'ln3_g': 1.0 + normal((L, D), 0.02),
        'ln3_b': normal((L, D), 0.02),
    }


def reference(x, mem, w_in, gdn_conv_w, gdn_a_log, gdn_dt_bias, gdn_norm_w, fox_f_bias,
              gla_w_gate2, gla_b_gate, gla_norm_w, p_gdn, p_fox, p_gla, b_merge, w_out,
              ln1_g, ln1_b, xa_wq, xa_wkv, xa_wo, ln2_g, ln2_b,
              moe_w_group, moe_b_group, moe_w_expert, moe_b_expert, moe_w_gate, moe_w_up, moe_w_down,
              ln3_g, ln3_b):
    for l in range(DEPTH):
        mix = hybrid_token_mixer(x, w_in[l], gdn_conv_w[l], gdn_a_log[l], gdn_dt_bias[l], gdn_norm_w[l],
                                 fox_f_bias[l], gla_w_gate2[l], gla_b_gate[l], gla_norm_w[l],
                                 p_gdn[l], p_fox[l], p_gla[l], b_merge[l], w_out[l])
        x = layer_norm(DEEPNORM_ALPHA * x + mix, ln1_g[l], ln1_b[l])
        xa = memory_cross_attention(x, mem, xa_wq[l], xa_wkv[l], xa_wo[l])
        x = layer_norm(DEEPNORM_ALPHA * x + xa, ln2_g[l], ln2_b[l])
        ff = hierarchical_moe(x, moe_w_group[l], moe_b_group[l], moe_w_expert[l], moe_b_expert[l],
                              moe_w_gate[l], moe_w_up[l], moe_w_down[l])
        x = layer_norm(DEEPNORM_ALPHA * x + ff, ln3_g[l], ln3_b[l])
    return x
```

```python
import numpy as np
import concourse.bass as bass
import concourse.mybir as mybir
from concourse.bass_utils import run_bass_kernel_spmd
from contextlib import ExitStack

F32 = mybir.dt.float32
BF16 = mybir.dt.bfloat16
AF = mybir.ActivationFunctionType
ALU = mybir.AluOpType

import os as _os
SAME_ENG_SYNC = _os.environ.get('SES', '1') == '1'
N_CORES = 8
DEPTH = 2
D = 1024
S = 2048
NT = 16
NSEQ = 2
NTOK = NSEQ * S
ALPHA = float((2 * DEPTH) ** 0.25)
OFF = dict(gdn_q=0, gdn_k=512, gdn_v=1024, gdn_b=1536, gdn_a=1540, gdn_z=1544, fox_q=2056, fox_k=2568,
           fox_v=3080, fox_f=3592, gla_q=3600, gla_k=3856, gla_v=4112, gla_r=4624, gla_lr=5136, merge=5152)
N_IN = 8224
NEG = -30000.0


class Prog:
    ENG = ('pe', 'act', 'dve', 'pool', 'sp')

    def __init__(self, nc, es):
        self.nc = nc
        self.es = es
        self.q = {e: [] for e in self.ENG}
        self.sems = {}
        self.cnt = {}
        self.seen = {e: {} for e in self.ENG}
        self.wr = {}
        self.rd = {}
        for e in ('pe', 'act', 'dve', 'pool'):
            self._sem('E_' + e)
        self.n_ops = 0

    def _sem(self, name):
        if name not in self.sems:
            self.sems[name] = self.es.enter_context(self.nc.semaphore(name))
            self.cnt[name] = 0
        return self.sems[name]

    @staticmethod
    def _norm(keys):
        out = []
        for k in keys:
            if isinstance(k, str) and len(k) == 4 and k[:2] == 'ps' and k[3] in 'bcd':
                k = k[:3]
            if k not in out:
                out.append(k)
        return tuple(out)

    def _collect(self, eng, reads, writes):
        need = {}

        def add(tok):
            for s, v in tok.items():
                if v > need.get(s, 0):
                    need[s] = v
        for r in reads:
            add(self.wr.get(r, {}))
            if isinstance(r, str) and r[:2] == 'ps' and len(r) == 3:
                add({s: v for s, v in self.rd.get(r, {}).items() if s != 'E_' + eng})
        for w in writes:
            add(self.wr.get(w, {}))
            add(self.rd.get(w, {}))
        waits = []
        own = 'E_' + eng
        for s, v in need.items():
            if s == own and (eng == 'pe' or not SAME_ENG_SYNC):
                continue
            if self.seen[eng].get(s, 0) >= v:
                continue
            self.seen[eng][s] = v
            waits.append((s, v))
        return waits

    def _commit(self, reads, writes, tok):
        for r in reads:
            d = self.rd.setdefault(r, {})
            for s, v in tok.items():
                if v > d.get(s, 0):
                    d[s] = v
        for w in writes:
            self.wr[w] = dict(tok)
            self.rd[w] = {}

    def op(self, eng, fn, reads=(), writes=(), inc=True):
        reads = self._norm(reads)
        writes = self._norm(writes)
        waits = self._collect(eng, reads, writes)
        s = 'E_' + eng
        if inc:
            self.cnt[s] += 1
            tok = {s: self.cnt[s]}
            self.q[eng].append((waits, fn, (s, 1)))
        else:
            tok = {s: self.cnt[s] + 1}
            self.q[eng].append((waits, fn, None))
        self._commit(reads, writes, tok)
        self.n_ops += 1

    def dma(self, out, in_, reads=(), writes=(), q='sp', key=None, **kw):
        reads = tuple(reads)
        writes = tuple(writes)
        waits = self._collect(q, reads, writes)
        if key is None:
            key = writes[0]
        s = ('W_' if q == 'pool' else 'D_') + str(key)
        self._sem(s)
        self.cnt[s] += 16
        tok = {s: self.cnt[s]}
        self.q[q].append((waits, lambda e: e.dma_start(out=out, in_=in_, **kw), (s, 16)))
        self._commit(reads, writes, tok)
        self.n_ops += 1

    def barrier(self):
        if _os.environ.get('NOBAR'):
            return
        for e in self.ENG:
            if e == _os.environ.get('BARSKIP'):
                continue
            waits = []
            for s, c in self.cnt.items():
                if c > 0 and self.seen[e].get(s, 0) < c:
                    if s == 'E_' + e:
                        continue
                    self.seen[e][s] = c
                    waits.append((s, c))
            if waits:
                self.q[e].append((waits, None, None))

    def finish(self):
        waits = []
        for s, c in self.cnt.items():
            if c > 0 and self.seen['sp'].get(s, 0) < c:
                waits.append((s, c))
        self.q['sp'].append((waits, None, None))

    def emit(self):
        nc = self.nc

        def run(name, e):
            for waits, fn, inc in self.q[name]:
                for s, v in waits:
                    e.wait_ge(self.sems[s], v)
                if fn is not None:
                    ins = fn(e)
                    if inc is not None:
                        ins.then_inc(self.sems[inc[0]], inc[1])
        with nc.Block() as block:
            @block.tensor
            def _(e):
                run('pe', e)

            @block.scalar
            def _(e):
                run('act', e)

            @block.vector
            def _(e):
                run('dve', e)

            @block.gpsimd
            def _(e):
                run('pool', e)

            @block.sync
            def _(e):
                run('sp', e)


class Builder:
    def __init__(self, debug=False, stages=('mix', 'xa', 'moe'), layers=(0, 1), nseq=NSEQ, sub=('gdn', 'fox', 'gla', 'merge')):
        self.debug = debug
        self.sub = sub
        self.stages = stages
        self.layers = layers
        self.nseq = nseq
        self.nc = bass.Bass("TRN2", target_bir_lowering=False)
        self.uid = 0
        self.names = {}

    def din(self, name, shape, dt=F32):
        return self.nc.dram_tensor(name, list(shape), dt, kind="ExternalInput").ap()

    def sb(self, es, name, shape, dt):
        self.uid += 1
        self.names[name] = f"{name}_{self.uid}"
        return es.enter_context(self.nc.sbuf_tensor(f"{name}_{self.uid}", list(shape), dt))

    def mm(self, out, lhsT, rhs, start=True, stop=True, rd=(), wr=(), skip=False, inc=None):
        kw = dict(start=start, stop=stop)
        if skip:
            kw['skip_group_check'] = True
        if inc is None:
            inc = stop or skip
        self.P.op('pe', lambda e: e.matmul(out, lhsT=lhsT, rhs=rhs, **kw), rd, wr, inc=inc)

    def tr(self, out, in_, rd=(), wr=()):
        idt = self.ident
        self.P.op('pe', lambda e: e.transpose(out=out, in_=in_, identity=idt[:]), tuple(rd) + ("const",), wr)

    def act(self, out, in_, func, bias=None, scale=None, rd=(), wr=(), accum=None):
        kw = {}
        if bias is not None:
            kw['bias'] = bias
        if scale is not None:
            kw['scale'] = scale
        if accum is not None:
            kw['accum_out'] = accum
        self.P.op('act', lambda e: e.activation(out=out, in_=in_, func=func, **kw), rd, wr)

    def ts(self, out, in0, s1, s2, op0, op1=None, rd=(), wr=(), eng='dve', accum=None):
        kw = {}
        if op1 is not None:
            kw['op1'] = op1
        if accum is not None:
            kw['accum_out'] = accum
        self.P.op(eng, lambda e: e.tensor_scalar(out=out, in0=in0, scalar1=s1, scalar2=s2, op0=op0, **kw), rd, wr)

    def tt(self, out, in0, in1, op, rd=(), wr=(), eng='dve'):
        self.P.op(eng, lambda e: e.tensor_tensor(out=out, in0=in0, in1=in1, op=op), rd, wr)

    def stt(self, out, in0, scalar, in1, op0, op1, rd=(), wr=()):
        self.P.op('dve', lambda e: e.scalar_tensor_tensor(out=out, in0=in0, scalar=scalar, in1=in1, op0=op0, op1=op1), rd, wr)

    def cp(self, out, in_, rd=(), wr=(), eng='dve'):
        if eng == 'act':
            self.P.op('act', lambda e: e.activation(out=out, in_=in_, func=AF.Copy), rd, wr)
        else:
            self.P.op(eng, lambda e: e.tensor_copy(out=out, in_=in_), rd, wr)

    def memset(self, ap, val, wr=(), eng='dve'):
        self.P.op(eng, lambda e: e.memset(ap, val), (), wr)

    def wcols(self, w_ap, l, c0, c1):
        return w_ap[l, :, c0:c1].rearrange("(k p) n -> p k n", p=128)

    def bcast(self, ap1d):
        return ap1d.partition_broadcast(128)

    @staticmethod
    def RK(i, r):
        return f"ps{i}" + ["", "b", "c", "d"][r]

    @staticmethod
    def BK(i):
        return (f"ps{i}", f"ps{i}b", f"ps{i}c", f"ps{i}d")

    def build(self):
        nc = self.nc
        dbg = self.debug
        I = {}
        I['x'] = self.din("x", [NTOK, D])
        I['mem'] = self.din("mem", [NSEQ * 256, D])
        I['w_in'] = self.din("w_in", [DEPTH, D, N_IN])
        I['conv_wT'] = self.din("conv_wT", [DEPTH, 128, 48])
        I['w_gate_r'] = self.din("w_gate_r", [DEPTH, 128, 256])
        I['moe_wr_r'] = self.din("moe_wr_r", [DEPTH, 128, 288])
        I['alog_r'] = self.din("alog_r", [DEPTH, 64])
        I['dtb_r'] = self.din("dtb_r", [DEPTH, 64])
        I['fb_r'] = self.din("fb_r", [DEPTH, 128])
        for n, shp in [('gdn_norm_w', [DEPTH, 128]),
                       ('gla_w_gate2', [DEPTH, 16, 256]), ('gla_b_gate', [DEPTH, 256]),
                       ('gla_norm_w', [DEPTH, 128]), ('p_gdn', [DEPTH, 512, D]), ('p_fox', [DEPTH, 512, D]),
                       ('p_gla', [DEPTH, 512, D]), ('b_merge', [DEPTH, 3072]), ('w_out', [DEPTH, D, D]),
                       ('ln1_g', [DEPTH, D]), ('ln1_b', [DEPTH, D]), ('xa_wq', [DEPTH, D, D]),
                       ('xa_wkv', [DEPTH, D, 2 * D]), ('xa_wo', [DEPTH, D, D]), ('ln2_g', [DEPTH, D]),
                       ('ln2_b', [DEPTH, D]), ('moe_br', [DEPTH, 36]),
                       ('moe_w_gate', [DEPTH, 32, D, 256]), ('moe_w_up', [DEPTH, 32, D, 256]),
                       ('moe_w_down', [DEPTH, 32, 256, D]), ('ln3_g', [DEPTH, D]), ('ln3_b', [DEPTH, D]),
                       ('c_ident', [128, 128]), ('c_tri', [128, 128]), ('c_ones', [128, 128]),
                       ('c_mneg_sl', [128, 128]), ('c_mneg_iu', [128, 128]), ('c_causal', [128, 128]),
                       ('c_sel', [32, 32 * 128])]:
            I[n] = self.din(n, shp)
        self.I = I
        kind = "ExternalOutput" if dbg else "Internal"
        self.out = nc.dram_tensor("out", [NTOK, D], F32, kind="ExternalOutput").ap()
        self.scr = [nc.dram_tensor(f"scr{i}", [NTOK, D], F32, kind=kind).ap() for i in range(3)]
        with ExitStack() as es:
            self.P = Prog(nc, es)
            self.ps = [es.enter_context(nc.psum_tensor(f"ps{i}", [128, 512], F32)) for i in range(8)]
            self.ident = self.sb(es, "ident", [128, 128], F32)
            self.identb = self.sb(es, "identb", [128, 128], BF16)
            self.tri = self.sb(es, "tri", [128, 128], F32)
            self.ones = self.sb(es, "ones", [128, 128], F32)
            self.mneg_sl = self.sb(es, "mneg_sl", [128, 128], BF16)
            self.mneg_iu = self.sb(es, "mneg_iu", [128, 128], BF16)
            self.causal = self.sb(es, "causal", [128, 128], F32)
            self.causalb = self.sb(es, "causalb", [128, 128], BF16)
            P = self.P
            P.dma(self.ident[:], I['c_ident'], writes=["const"], key="const")
            P.dma(self.tri[:], I['c_tri'], writes=["const"], key="const")
            P.dma(self.ones[:], I['c_ones'], writes=["const"], key="const")
            P.dma(self.causal[:], I['c_causal'], writes=["const"], key="const")
            if not _os.environ.get('NOPOOLC'):
                P.dma(self.identb[:], I['c_ident'], writes=["const"], key="const", q='pool')
                P.dma(self.mneg_sl[:], I['c_mneg_sl'], writes=["const"], key="const", q='pool')
                P.dma(self.mneg_iu[:], I['c_mneg_iu'], writes=["const"], key="const", q='pool')
                P.dma(self.causalb[:], I['c_causal'], writes=["const"], key="const", q='pool')
            self.xT = self.sb(es, "xT", [128, 8, S], BF16)

            for l in self.layers:
                src = I['x'] if l == 0 else self.scr[2]
                dst = self.out if l == DEPTH - 1 else self.scr[2]
                if 'mix' in self.stages:
                    for s in range(self.nseq):
                        self.mixer(l, s, src, self.scr[0])
                if 'xa' in self.stages:
                    for s in range(self.nseq):
                        self.xattn(l, s, self.scr[0] if 'mix' in self.stages else I['x'], self.scr[1])
                        P.barrier()
                if 'moe' in self.stages:
                    for s in range(self.nseq):
                        self.moe(l, s, self.scr[1] if 'xa' in self.stages else I['x'], dst)
                        P.barrier()
            P.finish()
            P.emit()
        return nc

    def alloc_io(self, es, ln=True, ytile=True, nxin=2):
        self.xin = [self.sb(es, f"xin{i}", [128, D], F32) for i in range(nxin)]
        if ln:
            self.lng = self.sb(es, "lng", [128, D], F32)
            self.lnb = self.sb(es, "lnb", [128, D], F32)
            self.lnst = self.sb(es, "lnst", [128, 2, 6], F32)
            self.lnmv = self.sb(es, "lnmv", [128, 2], F32)
            self.lnr = self.sb(es, "lnr", [128, 1], F32)
        if ytile:
            self.ytile = [self.sb(es, f"ytile{i}", [128, D], F32) for i in range(2)]

    def load_ln(self, gname, bname, l):
        P = self.P
        P.dma(self.lng[:], self.bcast(self.I[gname][l, :]), writes=["lng"])
        P.dma(self.lnb[:], self.bcast(self.I[bname][l, :]), writes=["lnb"])

    def resid_ln_store(self, yt, ykey, xres, xkey, dst_rows):
        P = self.P
        self.stt(yt[:], xres[:], ALPHA, yt[:], ALU.mult, ALU.add, rd=[ykey, xkey], wr=[ykey])
        for c in range(2):
            st = self.lnst
            P.op('dve', lambda e, c=c: e.bn_stats(out=st[:, c, :], in_=yt[:, c * 512:(c + 1) * 512]), [ykey], ["lnst"])
        st, mv, lr = self.lnst, self.lnmv, self.lnr
        P.op('dve', lambda e: e.bn_aggr(out=mv[:], in_=st[:].rearrange("p a b -> p (a b)")), ["lnst"], ["lnmv"])
        self.act(lr[:], mv[:, 1:2], AF.Ln, bias=1e-5, rd=["lnmv"], wr=["lnr"])
        self.act(lr[:], lr[:], AF.Exp, scale=-0.5, rd=["lnr"], wr=["lnr"])
        self.ts(yt[:], yt[:], mv[:, 0:1], lr[:, 0:1], ALU.subtract, ALU.mult, rd=[ykey, "lnmv", "lnr"], wr=[ykey])
        self.tt(yt[:], yt[:], self.lng[:], ALU.mult, rd=[ykey, "lng"], wr=[ykey], eng='pool')
        self.tt(yt[:], yt[:], self.lnb[:], ALU.add, rd=[ykey, "lnb"], wr=[ykey], eng='pool')
        P.dma(dst_rows, yt[:], reads=[ykey], writes=["dram_store"], key="st_" + ykey)

    def build_xT(self, src, row0, gate_w=None, gate_out=None, gate_n=0, xTf=None):
        P = self.P
        ps = self.ps
        import os
        for t in range(int(os.environ.get("NT_X", NT))):
            nx = len(self.xin)
            buf = self.xin[t % nx]
            bk = f"xin{t % nx}"
            P.dma(buf[:], src[row0 + t * 128: row0 + (t + 1) * 128, :], writes=[bk])
            for half in range(2):
                pk = self.BK(half)
                for kk in range(4):
                    k = half * 4 + kk
                    self.tr(ps[half][:, kk * 128:(kk + 1) * 128], buf[:, k * 128:(k + 1) * 128], rd=[bk], wr=[self.RK(half, kk)])
                pview = ps[half][:, :].rearrange("p (a b) -> p a b", a=4)
                self.cp(self.xT[:, half * 4:(half + 1) * 4, t * 128:(t + 1) * 128], pview, rd=pk, wr=[("xT", t)], eng='act')
                if xTf is not None:
                    self.cp(xTf[:, half * 4:(half + 1) * 4, :], pview, rd=pk, wr=["xTf"], eng='dve')
            if gate_w is not None:
                for k in range(8):
                    self.mm(ps[2][:, 0:gate_n], xTf[:, k, :], gate_w[:, k, :], start=(k == 0), stop=(k == 7),
                            rd=["xTf", "gate_w"], wr=["ps2"])
                self.cp(gate_out[:, t, :], ps[2][:, 0:gate_n], rd=["ps2"], wr=["graw"])

    def mixer(self, l, s, src, dst):
        P = self.P
        I = self.I
        ps = self.ps
        row0 = s * S
        XT = [("xT", t) for t in range(NT)]
        with ExitStack() as es:
            oT = self.sb(es, "oT", [128, 12, S], BF16)
            graw = self.sb(es, "graw", [128, NT, 32], F32)
            with ExitStack() as e0:
                self.alloc_io(e0, ln=False, ytile=False, nxin=4)
                xTf = self.sb(e0, "xTf", [128, 8, 128], F32)
                gate_w = self.sb(e0, "gate_w", [128, 8, 32], F32)
                P.dma(gate_w[:], I['w_gate_r'][l].rearrange("p (k n) -> p k n", k=8), writes=["gate_w"])
                if _os.environ.get('NOGATE'):
                    self.build_xT(src, row0)
                else:
                    self.build_xT(src, row0, gate_w, graw, 32, xTf)
                P.barrier()
            sub = self.sub
            if 'gdn' in sub:
                self.gdn(l, graw, oT, XT)
                P.barrier()
            if 'fox' in sub:
                self.fox(l, graw, oT, XT)
                P.barrier()
            if 'gla' in sub:
                self.gla(l, graw, oT, XT)
                P.barrier()
            if 'merge' in sub:
                self.merge(l, s, src, dst, oT, XT)
                P.barrier()

    def gdn(self, l, graw, oT, XT):
        P = self.P
        I = self.I
        ps = self.ps
        with ExitStack() as es:
            sb = lambda n, shp, dt=F32: self.sb(es, n, shp, dt)
            cw = sb("cw", [128, 12, 4])
            P.dma(cw[:], I['conv_wT'][l].rearrange("p (b i) -> p b i", b=12), writes=["cw"])
            nw = sb("nw", [128, 128])
            P.dma(nw[:], self.bcast(I['gdn_norm_w'][l, :]), writes=["nw"])
            alog = sb("alog", [128, NT, 4])
            dtb = sb("dtb", [128, NT, 4])
            P.dma(alog[:].rearrange("p a b -> p (a b)"), self.bcast(I['alog_r'][l, :]), writes=["alog"])
            P.dma(dtb[:].rearrange("p a b -> p (a b)"), self.bcast(I['dtb_r'][l, :]), writes=["dtb"])
            beta = sb("beta", [128, NT, 4])
            gg = sb("gg", [128, NT, 4])
            gc = sb("gc", [128, NT, 4])
            ngc = sb("ngc", [128, NT, 4])
            gtot = sb("gtot", [128, NT, 4])
            eg = sb("eg", [128, NT, 4])
            eqs = sb("eqs", [128, NT, 4])
            edec = sb("edec", [128, NT, 4])
            etot = sb("etot", [128, NT, 4])
            beg = sb("beg", [128, NT, 4])
            self.act(beta[:], graw[:, :, 0:4], AF.Sigmoid, rd=["graw"], wr=["beta"])
            self.tt(gg[:], graw[:, :, 4:8], dtb[:], ALU.add, rd=["graw", "dtb"], wr=["gg"])
            self.act(gg[:], gg[:], AF.Exp, rd=["gg"], wr=["gg"])
            self.act(gg[:], gg[:], AF.Ln, bias=1.0, rd=["gg"], wr=["gg"])
            self.act(alog[:], alog[:], AF.Exp, rd=["alog"], wr=["alog"])
            self.stt(gg[:], gg[:], -1.0, alog[:], ALU.mult, ALU.mult, rd=["gg", "alog"], wr=["gg"])
            for t in range(NT):
                self.mm(ps[2][:, 0:4], self.tri[:], gg[:, t, :], rd=["gg", "const"], wr=["ps2"])
                self.mm(ps[2][:, 4:8], self.ones[:], gg[:, t, :], rd=["gg", "const"], wr=["ps2"])
                self.cp(gc[:, t, :], ps[2][:, 0:4], rd=["ps2"], wr=["gc"])
                self.cp(gtot[:, t, :], ps[2][:, 4:8], rd=["ps2"], wr=["gtot"], eng='act')
            self.ts(ngc[:], gc[:], -1.0, None, ALU.mult, rd=["gc"], wr=["ngc"])
            self.act(eg[:], gc[:], AF.Exp, rd=["gc"], wr=["eg"])
            self.ts(eqs[:], eg[:], 128.0 ** -0.5, None, ALU.mult, rd=["eg"], wr=["eqs"])
            self.tt(edec[:], gtot[:], gc[:], ALU.subtract, rd=["gtot", "gc"], wr=["edec"])
            self.act(edec[:], edec[:], AF.Exp, rd=["edec"], wr=["edec"])
            self.act(etot[:], gtot[:], AF.Exp, rd=["gtot"], wr=["etot"])
            self.tt(beg[:], beta[:], eg[:], ALU.mult, rd=["beta", "eg"], wr=["beg"])
            GATES = ["beta", "gc", "ngc", "eg", "eqs", "edec", "etot", "beg"]
            wqkv = [sb(f"wqkv{i}", [128, 8, 3, 128], BF16) for i in range(2)]
            wz = [sb(f"wz{i}", [128, 8, 128], BF16) for i in range(2)]
            pre = sb("pre", [128, S + 4])
            qkvT = [sb(f"qkvT{j}", [128, S]) for j in range(3)]
            self.memset(pre[:, 0:3], 0.0, wr=["pre"])
            NTS, NHS, NRS = 4, 8, 2
            TS, HS, RS = [], [], []
            for b in range(NTS):
                d = {n: sb(f"T{n}{b}", [128, 128]) for n in ["dgn", "dgp", "ET", "E", "L0", "M0", "La", "Lb", "Ma", "Mb"]}
                d["Y0"] = sb(f"TY0{b}", [128, 256])
                TS.append(d)
            for b in range(NHS):
                d = {n: sb(f"H{n}{b}", [128, 128], BF16) for n in ["attnT", "kdec"]}
                d["wT"] = sb(f"HwT{b}", [128, 128])
                d["Y1"] = sb(f"HY1{b}", [128, 256])
                HS.append(d)
            for b in range(NRS):
                d = {n: sb(f"R{n}{b}", [128, 128]) for n in ["As", "ot"]}
                d["vnew"] = sb(f"Rvnew{b}", [128, 128], BF16)
                d["ss"] = sb(f"Rss{b}", [128, 1])
                RS.append(d)
            Sst = [sb(f"Sst{i}", [128, 128]) for i in range(2)]
            zs_all = sb("zs_all", [128, NT, 128])

            def load_head_w(h):
                i = h % 2
                for j, nm in enumerate(['gdn_q', 'gdn_k', 'gdn_v']):
                    P.dma(wqkv[i][:, :, j, :], self.wcols(I['w_in'], l, OFF[nm] + h * 128, OFF[nm] + (h + 1) * 128),
                          writes=[f"wqkv{i}"], q='pool')
                P.dma(wz[i][:], self.wcols(I['w_in'], l, OFF['gdn_z'] + h * 128, OFF['gdn_z'] + (h + 1) * 128),
                      writes=[f"wz{i}"], q='pool')
            load_head_w(0)
            for h in range(4):
                wi = h % 2
                if h + 1 < 4:
                    load_head_w(h + 1)
                for j in range(3):
                    for tg in range(4):
                        bank = ps[tg % 2]
                        pk = self.BK(tg % 2)
                        for k in range(8):
                            self.mm(bank[:, :], wqkv[wi][:, k, j, :], self.xT[:, k, tg * 512:(tg + 1) * 512],
                                    start=(k == 0), stop=(k == 7), rd=[f"wqkv{wi}"] + XT[tg * 4:(tg + 1) * 4], wr=pk)
                        self.cp(pre[:, 3 + tg * 512: 3 + (tg + 1) * 512], bank[:, :], rd=pk, wr=["pre"], eng='act')
                    dstT = qkvT[j]
                    dk_ = f"qkvT{j}"
                    blk = j * 4 + h
                    self.ts(dstT[:], pre[:, 0:S], cw[:, blk, 0:1], None, ALU.mult, rd=["pre", "cw"], wr=[dk_])
                    for i in range(1, 4):
                        self.stt(dstT[:], pre[:, i:i + S], cw[:, blk, i:i + 1], dstT[:], ALU.mult, ALU.add,
                                 rd=["pre", "cw", dk_], wr=[dk_])
                    self.act(dstT[:], dstT[:], AF.Silu, rd=[dk_], wr=[dk_])
                    if j < 2:
                        sq = pre[:, 4:4 + S]
                        self.act(sq, dstT[:], AF.Square, rd=[dk_], wr=["pre"])
                        for tg in range(4):
                            bank = ps[2 + tg % 2]
                            pk = self.BK(2 + tg % 2)
                            self.mm(bank[:, :], self.ones[:], sq[:, tg * 512:(tg + 1) * 512], rd=["pre", "const"], wr=pk)
                            self.act(sq[:, tg * 512:(tg + 1) * 512], bank[:, :], AF.Ln, bias=1e-6, rd=pk, wr=["pre"])
                        self.act(sq, sq, AF.Exp, scale=-0.5, rd=["pre"], wr=["pre"])
                        self.tt(dstT[:], dstT[:], sq, ALU.mult, rd=[dk_, "pre"], wr=[dk_])
                qT, kT, vT = qkvT
                self.memset(Sst[0][:], 0.0, wr=["Sst0"])
                for t in range(NT):
                    zb = ps[4 + t % 2]
                    for k in range(8):
                        self.mm(zb[:, 0:128], self.xT[:, k, t * 128:(t + 1) * 128], wz[wi][:, k, :], start=(k == 0), stop=(k == 7),
                                rd=[XT[t], f"wz{wi}"], wr=[f"ps{4 + t % 2}"])
                    self.cp(zs_all[:, t, :], zb[:, 0:128], rd=[f"ps{4 + t % 2}"], wr=["zs_all"], eng='dve')
                self.act(zs_all[:], zs_all[:], AF.Silu, rd=["zs_all"], wr=["zs_all"])

                def pre_gen(c, h=h, qT=qT, kT=kT, vT=vT):
                    T = TS[c % NTS]
                    H = HS[c % NHS]
                    tb, hb = c % NTS, c % NHS
                    TK = lambda n: f"T{n}{tb}"
                    HK = lambda n: f"H{n}{hb}"
                    cs = slice(c * 128, (c + 1) * 128)
                    col = lambda tile: tile[:, c, h:h + 1]
                    pA, pB, pC = ps[c % 2], ps[2 + c % 2], ps[4 + c % 2]
                    kA, kB, kC = f"ps{c % 2}", f"ps{2 + c % 2}", f"ps{4 + c % 2}"
                    self.ts(T["dgn"][:], self.ident[:], col(ngc), None, ALU.mult, rd=["const", "ngc"], wr=[TK("dgn")])
                    self.ts(T["dgp"][:], self.ident[:], col(gc), None, ALU.mult, rd=["const", "gc"], wr=[TK("dgp")], eng='pool')
                    yield
                    self.mm(pA[:, 0:128], self.ones[:], T["dgn"][:], start=True, stop=False, rd=["const", TK("dgn")], wr=[kA])
                    self.mm(pA[:, 0:128], self.identb[:], self.mneg_sl[:], start=False, stop=True, rd=["const"], wr=[kA])
                    self.mm(pA[:, 128:256], self.ones[:], T["dgp"][:], start=True, stop=False, rd=["const", TK("dgp")], wr=[kA])
                    self.mm(pA[:, 128:256], self.identb[:], self.mneg_iu[:], start=False, stop=True, rd=["const"], wr=[kA])
                    self.mm(pA[:, 256:384], kT[:, cs], kT[:, cs], rd=["qkvT1"], wr=[kA])
                    self.mm(pA[:, 384:512], kT[:, cs], qT[:, cs], rd=["qkvT0", "qkvT1"], wr=[kA])
                    self.act(T["ET"][:], pA[:, 0:128], AF.Exp, bias=col(gc), rd=[kA, "gc"], wr=[TK("ET")])
                    self.act(T["E"][:], pA[:, 128:256], AF.Exp, bias=col(ngc), rd=[kA, "ngc"], wr=[TK("E")])
                    self.stt(T["L0"][:], pA[:, 256:384], col(beta), T["ET"][:], ALU.mult, ALU.mult, rd=[kA, "beta", TK("ET")], wr=[TK("L0")])
                    self.stt(H["attnT"][:], pA[:, 384:512], 128.0 ** -0.5, T["E"][:], ALU.mult, ALU.mult, rd=[kA, TK("E")], wr=[HK("attnT")])
                    yield
                    self.tr(pB[:, 0:128], T["L0"][:], rd=[TK("L0")], wr=[kB])
                    self.tr(pB[:, 128:256], kT[:, cs], rd=["qkvT1"], wr=[kB])
                    self.tr(pB[:, 256:384], vT[:, cs], rd=["qkvT2"], wr=[kB])
                    self.cp(T["M0"][:], pB[:, 0:128], rd=[kB], wr=[TK("M0")], eng='act')
                    self.act(H["kdec"][:], pB[:, 128:256], AF.Copy, scale=col(edec), rd=[kB, "edec"], wr=[HK("kdec")])
                    Y0, Y1 = T["Y0"], H["Y1"]
                    self.ts(Y0[:, 128:256], pB[:, 128:256], col(beg), None, ALU.mult, rd=[kB, "beg"], wr=[TK("Y0")])
                    self.ts(Y0[:, 0:128], pB[:, 256:384], col(beta), None, ALU.mult, rd=[kB, "beta"], wr=[TK("Y0")])
                    yield
                    Ys = [(Y0, TK("Y0")), (Y1, HK("Y1"))]
                    yi = 0
                    Lc, Mc, Lk, Mk = T["L0"], T["M0"], TK("L0"), TK("M0")
                    nxt = [(T["La"], T["Ma"], TK("La"), TK("Ma")), (T["Lb"], T["Mb"], TK("Lb"), TK("Mb"))]
                    for lev in range(7):
                        (Yc, Yck), (Yn, Ynk) = Ys[yi], Ys[1 - yi]
                        self.mm(pB[:, 0:256], Mc[:], Yc[:], rd=[Mk, Yck], wr=[kB])
                        if lev < 6:
                            Ln_, Mn_, Lnk, Mnk = nxt[lev % 2]
                            self.mm(pC[:, 0:128], Lc[:], Mc[:], rd=[Lk, Mk], wr=[kC])
                            if lev < 5:
                                self.mm(pC[:, 128:256], Mc[:], Lc[:], rd=[Lk, Mk], wr=[kC])
                        self.tt(Yn[:], Yc[:], pB[:, 0:256], ALU.subtract if lev == 0 else ALU.add, rd=[Yck, kB], wr=[Ynk])
                        if lev < 6:
                            self.cp(Mn_[:], pC[:, 0:128], rd=[kC], wr=[Mnk], eng='act')
                            if lev < 5:
                                self.cp(Ln_[:], pC[:, 128:256], rd=[kC], wr=[Lnk], eng='act')
                            Lc, Mc, Lk, Mk = Ln_, Mn_, Lnk, Mnk
                        yi = 1 - yi
                        yield
                    assert yi == 1
                    self.tr(pC[:, 256:384], Y1[:, 128:256], rd=[HK("Y1")], wr=[kC])
                    self.cp(H["wT"][:], pC[:, 256:384], rd=[kC], wr=[HK("wT")], eng='act')
                    yield

                def rec_gen(c, h=h, qT=qT, wi=wi):
                    H = HS[c % NHS]
                    R = RS[c % NRS]
                    hb, rb = c % NHS, c % NRS
                    HK = lambda n: f"H{n}{hb}"
                    RK_ = lambda n: f"R{n}{rb}"
                    cs = slice(c * 128, (c + 1) * 128)
                    col = lambda tile: tile[:, c, h:h + 1]
                    Sc, Sn = Sst[c % 2], Sst[(c + 1) % 2]
                    Sck, Snk = f"Sst{c % 2}", f"Sst{(c + 1) % 2}"
                    Y1 = H["Y1"]
                    self.mm(ps[6][:, 0:128], H["wT"][:], Sc[:], rd=[HK("wT"), Sck], wr=["ps6"])
                    self.mm(ps[6][:, 128:256], qT[:, cs], Sc[:], rd=["qkvT0", Sck], wr=["ps6"])
                    self.tt(R["vnew"][:], Y1[:, 0:128], ps[6][:, 0:128], ALU.subtract, rd=[HK("Y1"), "ps6"], wr=[RK_("vnew")])
                    self.act(R["As"][:], ps[6][:, 128:256], AF.Copy, scale=col(eqs), rd=["ps6", "eqs"], wr=[RK_("As")])
                    yield
                    self.mm(ps[6][:, 384:512], H["kdec"][:], R["vnew"][:], rd=[HK("kdec"), RK_("vnew")], wr=["ps6"])
                    self.mm(ps[6][:, 256:384], H["attnT"][:], R["vnew"][:], rd=[HK("attnT"), RK_("vnew")], wr=["ps6"])
                    self.stt(Sn[:], Sc[:], col(etot), ps[6][:, 384:512], ALU.mult, ALU.add, rd=[Sck, "etot", "ps6"], wr=[Snk])
                    self.tt(R["ot"][:], R["As"][:], ps[6][:, 256:384], ALU.add, rd=[RK_("As"), "ps6"], wr=[RK_("ot")])
                    yield
                    for _ in self.out_gate_gen(R, RK_, zs_all[:, c, :], "zs_all", nw, "nw", oT[:, h, cs], ("oT", h, c)):
                        yield

                def rec_chain(cs_):
                    for c in cs_:
                        yield from rec_gen(c)

                def run_rr(gens):
                    gens = list(gens)
                    while gens:
                        for g in list(gens):
                            try:
                                next(g)
                            except StopIteration:
                                gens.remove(g)
                G = 4
                groups = [list(range(g0, g0 + G)) for g0 in range(0, NT, G)]
                run_rr([pre_gen(c) for c in groups[0]])
                for gi in range(len(groups)):
                    gens = []
                    if gi + 1 < len(groups):
                        gens += [pre_gen(c) for c in groups[gi + 1]]
                    gens.append(rec_chain(groups[gi]))
                    run_rr(gens)

    def out_gate_gen(self, d, K, zs_ap, zsk, nw, nwk, oT_dst, oTk, pbank=7, pcol=128):
        ps = self.ps
        pk = f"ps{pbank}"
        self.act(d["As"][:], d["ot"][:], AF.Square, rd=[K("ot"), K("As")], wr=[K("As"), K("ss")], accum=d["ss"][:])
        self.act(d["ss"][:], d["ss"][:], AF.Ln, bias=1e-6, scale=1.0 / 128.0, rd=[K("ss")], wr=[K("ss")])
        self.act(d["ss"][:], d["ss"][:], AF.Exp, scale=-0.5, rd=[K("ss")], wr=[K("ss")])
        yield
        self.stt(d["ot"][:], d["ot"][:], d["ss"][:, 0:1], nw[:], ALU.mult, ALU.mult, rd=[K("ot"), K("ss"), nwk], wr=[K("ot")])
        self.tt(d["ot"][:], d["ot"][:], zs_ap, ALU.mult, rd=[K("ot"), zsk], wr=[K("ot")], eng='pool')
        yield
        self.tr(ps[pbank][:, pcol:pcol + 128], d["ot"][:], rd=[K("ot")], wr=[pk])
        self.cp(oT_dst, ps[pbank][:, pcol:pcol + 128], rd=[pk], wr=[oTk], eng='act')
        yield

    def fox(self, l, graw, oT, XT):
        P = self.P
        I = self.I
        ps = self.ps
        BK = self.BK
        with ExitStack() as es:
            sb = lambda n, shp, dt=F32: self.sb(es, n, shp, dt)
            wv = sb("wv", [128, 8, 512], BF16)
            P.dma(wv[:], self.wcols(I['w_in'], l, OFF['fox_v'], OFF['fox_v'] + 512), writes=["wv"], q='pool')
            wqk = [sb(f"wqk{i}", [128, 8, 2, 128], BF16) for i in range(2)]

            def load_w(hp):
                i = hp % 2
                for j, nm in enumerate(['fox_q', 'fox_k']):
                    P.dma(wqk[i][:, :, j, :], self.wcols(I['w_in'], l, OFF[nm] + hp * 128, OFF[nm] + (hp + 1) * 128),
                          writes=[f"wqk{i}"], q='pool')
            load_w(0)
            fb = sb("fb", [128, NT, 8])
            P.dma(fb[:].rearrange("p a b -> p (a b)"), self.bcast(I['fb_r'][l, :]), writes=["fb"])
            vaug = sb("vaug", [128, NT, 8, 65], BF16)
            self.memset(vaug[:, :, :, 64:65], 1.0, wr=["vaug"])
            for t in range(NT):
                bank = ps[t % 2]
                pk = BK(t % 2)
                for k in range(8):
                    self.mm(bank[:, :], self.xT[:, k, t * 128:(t + 1) * 128], wv[:, k, :], start=(k == 0), stop=(k == 7),
                            rd=[XT[t], "wv"], wr=pk)
                self.cp(vaug[:, t, :, 0:64], bank[:, :].rearrange("p (a b) -> p a b", a=8), rd=pk, wr=["vaug"],
                        eng='act' if t % 2 else 'dve')
            logf = sb("logf", [128, NT, 8])
            lsum = sb("lsum", [128, NT, 8])
            cc = sb("cc", [128, NT, 8])
            cref = sb("cref", [128, NT, 8])
            self.tt(logf[:], graw[:, :, 8:16], fb[:], ALU.add, rd=["graw", "fb"], wr=["logf"])
            self.act(logf[:], logf[:], AF.Exp, scale=-1.0, rd=["logf"], wr=["logf"])
            self.act(logf[:], logf[:], AF.Ln, bias=1.0, rd=["logf"], wr=["logf"])
            self.ts(logf[:], logf[:], -1.0, None, ALU.mult, rd=["logf"], wr=["logf"])
            self.cp(lsum[:, 0, :], logf[:, 0, :], rd=["logf"], wr=["lsum"])
            for t in range(1, NT):
                self.tt(lsum[:, t, :], lsum[:, t - 1, :], logf[:, t, :], ALU.add, rd=["logf", "lsum"], wr=["lsum"])
            self.memset(cref[:, 0, :], 0.0, wr=["cref"])
            for t in range(NT):
                self.mm(ps[2][:, 0:8], self.tri[:], logf[:, t, :], start=True, stop=(t == 0), rd=["logf", "const"], wr=["ps2"])
                if t > 0:
                    self.mm(ps[2][:, 0:8], self.ones[:], lsum[:, t - 1, :], start=False, stop=True, rd=["lsum", "const"], wr=["ps2"])
                    self.mm(ps[2][:, 8:16], self.ones[:], lsum[:, t - 1, :], rd=["lsum", "const"], wr=["ps2"])
                    self.cp(cref[:, t, :], ps[2][:, 8:16], rd=["ps2"], wr=["cref"], eng='act')
                self.cp(cc[:, t, :], ps[2][:, 0:8], rd=["ps2"], wr=["cc"])
            biasm = sb("biasm", [128, 8, NT, NT])
            for h in range(8):
                for i in range(NT):
                    self.ts(biasm[:, h, i, 0:i + 1], cc[:, 0:i + 1, h], -1.0, cref[:, i, h:h + 1], ALU.mult, ALU.add,
                            rd=["cc", "cref"], wr=["biasm"])
            qkT = [sb(f"qkT{i}", [128, 2, S], BF16) for i in range(2)]
            ptile = [sb(f"ptile{i}", [128, 512], BF16) for i in range(3)]
            opair = sb("opair", [128, NT, 128])
            rinv = [sb(f"rinv{i}", [128, 1]) for i in range(2)]
            pcount = 0
            ocount = 0
            for hp in range(4):
                wi = hp % 2
                if hp + 1 < 4:
                    load_w(hp + 1)
                for j in range(2):
                    for tg in range(4):
                        bank = ps[tg % 2]
                        pk = BK(tg % 2)
                        for k in range(8):
                            self.mm(bank[:, :], wqk[wi][:, k, j, :], self.xT[:, k, tg * 512:(tg + 1) * 512],
                                    start=(k == 0), stop=(k == 7), rd=[f"wqk{wi}"] + XT[tg * 4:(tg + 1) * 4], wr=pk)
                        self.cp(qkT[wi][:, j, tg * 512:(tg + 1) * 512], bank[:, :], rd=pk, wr=[f"qkT{wi}"],
                                eng='act' if tg % 2 else 'dve')
                for hl in range(2):
                    h = hp * 2 + hl
                    prt = slice(hl * 64, (hl + 1) * 64)
                    for g in range(4):
                        ob = ps[4 + (g % 2)]
                        obk = BK(4 + (g % 2))
                        self.memset(ob[:, 0:260], 0.0, wr=obk)
                        def emit_qk(j, g=g, h=h, prt=prt, wi=wi):
                            nonlocal pcount
                            i_lo = max(j, 4 * g)
                            ncol = (4 * g + 4 - i_lo) * 128
                            sbank = ps[2 + (j % 2)]
                            sk = BK(2 + (j % 2))
                            self.mm(sbank[:, 0:ncol], qkT[wi][prt, 1, j * 128:(j + 1) * 128],
                                    qkT[wi][prt, 0, i_lo * 128:(4 * g + 4) * 128], rd=[f"qkT{wi}"], wr=sk)
                            pt = ptile[pcount % 3]
                            ptk = f"ptile{pcount % 3}"
                            pcount += 1
                            for i in range(i_lo, 4 * g + 4):
                                o = (i - i_lo) * 128
                                self.act(pt[:, o:o + 128], sbank[:, o:o + 128], AF.Exp, bias=biasm[:, h, i, j:j + 1], scale=0.125,
                                         rd=list(sk) + ["biasm"], wr=[(ptk, o // 128)])
                            if j >= 4 * g:
                                self.tt(pt[:, 0:128], pt[:, 0:128], self.causalb[:], ALU.mult, rd=[(ptk, 0), "const"], wr=[(ptk, 0)], eng='pool')
                            return pt, ptk, i_lo

                        def emit_pv(j, pt, ptk, i_lo, g=g, h=h, ob=ob, obk=obk):
                            for i in range(i_lo, 4 * g + 4):
                                o = (i - i_lo) * 128
                                oc = (i - 4 * g) * 65
                                self.mm(ob[:, oc:oc + 65], pt[:, o:o + 128], vaug[:, j, h, :], start=False, stop=False,
                                        rd=[(ptk, o // 128), "vaug"], wr=obk, skip=True, inc=(i == 4 * g + 3))
                        nj = 4 * g + 4
                        cur = emit_qk(0)
                        for j in range(nj):
                            nxt_ = emit_qk(j + 1) if j + 1 < nj else None
                            emit_pv(j, *cur)
                            cur = nxt_
                        for ii in range(4):
                            i = 4 * g + ii
                            oc = ii * 65
                            ri = rinv[ocount % 2]
                            rik = f"rinv{ocount % 2}"
                            ocount += 1
                            P.op('dve', lambda e, ri=ri, ob=ob, oc=oc: e.reciprocal(out=ri[:], in_=ob[:, oc + 64:oc + 65]), obk, [rik])
                            self.ts(opair[:, i, hl * 64:(hl + 1) * 64], ob[:, oc:oc + 64], ri[:, 0:1], None, ALU.mult,
                                    rd=list(obk) + [rik], wr=[("opair", i)])
                for t in range(NT):
                    r = t % 4
                    self.tr(ps[7][:, r * 128:(r + 1) * 128], opair[:, t, :], rd=[("opair", t)], wr=[self.RK(7, r)])
                    self.cp(oT[:, 4 + hp, t * 128:(t + 1) * 128], ps[7][:, r * 128:(r + 1) * 128], rd=[self.RK(7, r)],
                            wr=[("oT", 4 + hp, t)], eng='act')

    def gla(self, l, graw, oT, XT):
        P = self.P
        I = self.I
        ps = self.ps
        BK = self.BK
        with ExitStack() as es:
            sb = lambda n, shp, dt=F32: self.sb(es, n, shp, dt)
            wq = sb("wq", [128, 8, 256], BF16)
            wk = sb("wk", [128, 8, 256], BF16)
            wv = sb("wv", [128, 8, 512], BF16)
            wr_ = sb("wr", [128, 8, 512], BF16)
            for tl, nm, n in [(wq, 'gla_q', 256), (wk, 'gla_k', 256), (wv, 'gla_v', 512), (wr_, 'gla_r', 512)]:
                P.dma(tl[:], self.wcols(I['w_in'], l, OFF[nm], OFF[nm] + n), writes=["glaw"], q='pool')
            w2 = sb("w2", [16, 256])
            P.dma(w2[:], I['gla_w_gate2'][l], writes=["w2"])
            bg = sb("bg", [128, 256])
            P.dma(bg[:], self.bcast(I['gla_b_gate'][l, :]), writes=["bg"])
            nw = sb("nw", [128, 128])
            P.dma(nw[:], self.bcast(I['gla_norm_w'][l, :]), writes=["nwc"])
            lrT = sb("lrT", [16, 128])
            NB = 3
            bufs = []
            for b_ in range(NB):
                d = {}
                for n in ["la", "cum", "ecum", "encum", "edk", "qt", "kt", "kd"]:
                    d[n] = sb(f"{n}{b_}", [128, 256])
                d["v"] = sb(f"v{b_}", [128, 512])
                d["zs4"] = sb(f"zs4{b_}", [128, 512])
                d["qtT"] = sb(f"qtT{b_}", [128, 2, 128])
                d["ktT"] = sb(f"ktT{b_}", [128, 2, 128])
                d["cd"] = sb(f"cd{b_}", [128, 2])
                bufs.append(d)
            RB = []
            for b_ in range(4):
                d = {n: sb(f"gR{n}{b_}", [128, 128]) for n in ["attnT", "ot", "As"]}
                d["ss"] = sb(f"gRss{b_}", [128, 1])
                RB.append(d)
            Sg = [[sb(f"Sg{p}_{i}", [128, 128]) for i in range(2)] for p in range(2)]
            for p in range(2):
                self.memset(Sg[p][0][:], 0.0, wr=[f"Sg{p}_0_0", f"Sg{p}_1_0"])

            def pre_gen(c):
                d = bufs[c % NB]
                b_ = c % NB
                K = lambda n: f"g{n}{b_}"
                cs = slice(c * 128, (c + 1) * 128)
                self.tr(ps[0][0:16, 0:128], graw[:, c, 16:32], rd=["graw"], wr=["ps0"])
                self.cp(lrT[:], ps[0][0:16, 0:128], rd=["ps0"], wr=["lrT"])
                self.mm(ps[0][:, 256:512], lrT[:], w2[:], rd=["lrT", "w2"], wr=["ps0"])
                self.tt(d["la"][:], ps[0][:, 256:512], bg[:], ALU.add, rd=["ps0", "bg"], wr=[K("la")])
                yield
                self.act(d["la"][:], d["la"][:], AF.Exp, scale=-1.0, rd=[K("la")], wr=[K("la")])
                self.act(d["la"][:], d["la"][:], AF.Ln, bias=1.0, rd=[K("la")], wr=[K("la")])
                self.ts(d["la"][:], d["la"][:], -1.0 / 16.0, None, ALU.mult, rd=[K("la")], wr=[K("la")])
                yield
                self.mm(ps[1][:, 0:256], self.tri[:], d["la"][:], rd=["const", K("la")], wr=["ps1"])
                self.mm(ps[1][:, 256:512], self.ones[:], d["la"][:], rd=["const", K("la")], wr=["ps1"])
                for p in range(2):
                    self.mm(ps[2][:, p:p + 1], d["la"][:, p * 128:(p + 1) * 128], self.ones[:, 0:1], rd=[K("la"), "const"], wr=["ps2"])
                self.cp(d["cum"][:], ps[1][:, 0:256], rd=["ps1"], wr=[K("cum")])
                self.tt(d["edk"][:], ps[1][:, 256:512], d["cum"][:], ALU.subtract, rd=["ps1", K("cum")], wr=[K("edk")])
                self.act(d["cd"][:], ps[2][:, 0:2], AF.Exp, rd=["ps2"], wr=[K("cd")])
                yield
                self.act(d["ecum"][:], d["cum"][:], AF.Exp, rd=[K("cum")], wr=[K("ecum")])
                self.act(d["encum"][:], d["cum"][:], AF.Exp, scale=-1.0, rd=[K("cum")], wr=[K("encum")])
                self.act(d["edk"][:], d["edk"][:], AF.Exp, rd=[K("edk")], wr=[K("edk")])
                yield
                for k in range(8):
                    self.mm(ps[3][:, 0:256], self.xT[:, k, cs], wq[:, k, :], start=(k == 0), stop=(k == 7), rd=[XT[c], "glaw"], wr=["ps3"])
                for k in range(8):
                    self.mm(ps[3][:, 256:512], self.xT[:, k, cs], wk[:, k, :], start=(k == 0), stop=(k == 7), rd=[XT[c], "glaw"], wr=["ps3"])
                self.stt(d["qt"][:], ps[3][:, 0:256], 0.125, d["ecum"][:], ALU.mult, ALU.mult, rd=["ps3", K("ecum")], wr=[K("qt")])
                self.tt(d["kt"][:], ps[3][:, 256:512], d["encum"][:], ALU.mult, rd=["ps3", K("encum")], wr=[K("kt")])
                self.tt(d["kd"][:], ps[3][:, 256:512], d["edk"][:], ALU.mult, rd=["ps3", K("edk")], wr=[K("kd")])
                yield
                for k in range(8):
                    self.mm(ps[4][:, :], self.xT[:, k, cs], wv[:, k, :], start=(k == 0), stop=(k == 7), rd=[XT[c], "glaw"], wr=["ps4"])
                self.cp(d["v"][:], ps[4][:, :], rd=["ps4"], wr=[K("v")], eng='act')
                yield
                for k in range(8):
                    self.mm(ps[4][:, :], self.xT[:, k, cs], wr_[:, k, :], start=(k == 0), stop=(k == 7), rd=[XT[c], "glaw"], wr=["ps4"])
                self.act(d["zs4"][:], ps[4][:, :], AF.Silu, rd=["ps4"], wr=[K("zs4")])
                yield
                for p in range(2):
                    self.tr(ps[5][:, p * 128:(p + 1) * 128], d["qt"][:, p * 128:(p + 1) * 128], rd=[K("qt")], wr=["ps5"])
                    self.tr(ps[5][:, (2 + p) * 128:(3 + p) * 128], d["kt"][:, p * 128:(p + 1) * 128], rd=[K("kt")], wr=["ps5"])
                self.cp(d["qtT"][:], ps[5][:, 0:256].rearrange("p (a b) -> p a b", a=2), rd=["ps5"], wr=[K("qtT")], eng='act')
                self.cp(d["ktT"][:], ps[5][:, 256:512].rearrange("p (a b) -> p a b", a=2), rd=["ps5"], wr=[K("ktT")])
                yield

            def rec_head_gen(c, h):
                d = bufs[c % NB]
                b_ = c % NB
                K = lambda n: f"g{n}{b_}"
                cs = slice(c * 128, (c + 1) * 128)
                p, hl = h // 2, h % 2
                prt = slice(hl * 64, (hl + 1) * 64)
                R = RB[h]
                RK_ = lambda n: f"gR{n}{h}"
                Sc, Sn = Sg[p][c % 2], Sg[p][(c + 1) % 2]
                Sck, Snk = f"Sg{p}_{hl}_{c % 2}", f"Sg{p}_{hl}_{(c + 1) % 2}"
                pb = ps[6 + h % 2]
                pbk = f"ps{6 + h % 2}"
                self.mm(pb[:, 0:128], d["ktT"][prt, p, :], d["qtT"][prt, p, :], rd=[K("ktT"), K("qtT")], wr=[pbk])
                self.mm(pb[prt, 256:384], d["kd"][:, h * 64:(h + 1) * 64], d["v"][:, h * 128:(h + 1) * 128],
                        rd=[K("kd"), K("v")], wr=[pbk])
                self.tt(R["attnT"][:], pb[:, 0:128], self.causal[:], ALU.mult, rd=[pbk, "const"], wr=[RK_("attnT")])
                self.stt(Sn[prt, :], Sc[prt, :], d["cd"][prt, p:p + 1], pb[prt, 256:384], ALU.mult, ALU.add,
                         rd=[Sck, K("cd"), pbk], wr=[Snk])
                yield
                self.mm(pb[:, 128:256], d["qtT"][prt, p, :], Sc[prt, :], start=True, stop=False, rd=[K("qtT"), Sck], wr=[pbk])
                self.mm(pb[:, 128:256], R["attnT"][:], d["v"][:, h * 128:(h + 1) * 128], start=False, stop=True,
                        rd=[RK_("attnT"), K("v")], wr=[pbk])
                self.cp(R["ot"][:], pb[:, 128:256], rd=[pbk], wr=[RK_("ot")], eng='act')
                yield
                for _ in self.out_gate_gen(R, RK_, d["zs4"][:, h * 128:(h + 1) * 128], K("zs4"), nw, "nwc",
                                           oT[:, 8 + h, cs], ("oT", 8 + h, c), pbank=6 + h % 2, pcol=384):
                    yield

            def run_rr(gens):
                gens = list(gens)
                while gens:
                    for g in list(gens):
                        try:
                            next(g)
                        except StopIteration:
                            gens.remove(g)
            run_rr([pre_gen(0), pre_gen(1)])
            for c in range(NT):
                gens = [rec_head_gen(c, h) for h in range(4)]
                if c + 2 < NT:
                    gens.append(pre_gen(c + 2))
                run_rr(gens)

    def merge(self, l, s, src, dst, oT, XT):
        P = self.P
        I = self.I
        ps = self.ps
        BK = self.BK
        row0 = s * S
        with ExitStack() as es:
            sb = lambda n, shp, dt=F32: self.sb(es, n, shp, dt)
            self.alloc_io(es)
            wm = [sb(f"wm{i}", [128, 8, 512], BF16) for i in range(2)]
            P.dma(wm[0][:], self.wcols(I['w_in'], l, OFF['merge'], OFF['merge'] + 512), writes=["wm0"], q='pool')
            pw = [sb(f"pw{j}", [128, 4, D], BF16) for j in range(3)]
            for j, nm in enumerate(['p_gdn', 'p_fox', 'p_gla']):
                P.dma(pw[j][:], I[nm][l].rearrange("(c p) n -> p c n", p=128), writes=["pw"], q='pool')
            wo = sb("wo", [128, 8, D], BF16)
            P.dma(wo[:], I['w_out'][l].rearrange("(k p) n -> p k n", p=128), writes=["wo"], q='pool')
            bm = sb("bm", [128, 3 * D])
            P.dma(bm[:], self.bcast(I['b_merge'][l, :]), writes=["bm"])
            self.load_ln('ln1_g', 'ln1_b', l)
            merged = sb("merged", [128, 4, D])
            gt = [sb(f"gt{i}", [128, 512]) for i in range(2)]
            mT4 = [sb(f"mT4_{i}", [128, 8, 128], BF16) for i in range(4)]
            pendingB = []
            blocks = [(j, half) for half in range(2) for j in range(3)]
            nload = 0

            def load_wm(j, half):
                nonlocal nload
                i = nload % 2
                nload += 1
                c0 = OFF['merge'] + j * D + half * 512
                P.dma(wm[i][:], self.wcols(I['w_in'], l, c0, c0 + 512), writes=[f"wm{i}"], q='pool')
            seq = [(tg, j, half) for tg in range(4) for (j, half) in blocks]
            nload = 1
            cnt = 0
            for idx, (tg, j, half) in enumerate(seq):
                wi = idx % 2
                if idx + 1 < len(seq):
                    load_wm(seq[idx + 1][1], seq[idx + 1][2])
                for tt_ in range(4):
                    t = tg * 4 + tt_
                    cs = slice(t * 128, (t + 1) * 128)
                    gb = ps[cnt % 2]
                    gbk = BK(cnt % 2)
                    bb = ps[2 + cnt % 2]
                    bbk = BK(2 + cnt % 2)
                    g_ = gt[cnt % 2]
                    gk = f"gt{cnt % 2}"
                    cnt += 1
                    for k in range(8):
                        self.mm(gb[:, :], self.xT[:, k, cs], wm[wi][:, k, :], start=(k == 0), stop=(k == 7), rd=[XT[t], f"wm{wi}"], wr=gbk)
                    for c4 in range(4):
                        self.mm(bb[:, :], oT[:, 4 * j + c4, cs], pw[j][:, c4, half * 512:(half + 1) * 512], start=(c4 == 0), stop=(c4 == 3),
                                rd=[("oT", 4 * j + c4, t), "pw"], wr=bbk)
                    self.tt(g_[:], gb[:, :], bm[:, j * D + half * 512: j * D + (half + 1) * 512], ALU.add, rd=list(gbk) + ["bm"], wr=[gk])
                    self.act(g_[:], g_[:], AF.Sigmoid, rd=[gk], wr=[gk])
                    mslice = merged[:, tt_, half * 512:(half + 1) * 512]
                    if j == 0:
                        self.tt(mslice, g_[:], bb[:, :], ALU.mult, rd=[gk] + list(bbk), wr=[("merged", tt_)])
                    else:
                        self.tt(g_[:], g_[:], bb[:, :], ALU.mult, rd=[gk] + list(bbk), wr=[gk])
                        self.tt(mslice, mslice, g_[:], ALU.add, rd=[gk, ("merged", tt_)], wr=[("merged", tt_)], eng='pool')
                if pendingB:
                    tt2, t2 = pendingB.pop(0)
                    self.finish_B(mT4[tt2], f"mT4_{tt2}", wo, "wo", src, dst, row0 + t2 * 128, t2)
                if (j, half) == blocks[-1]:
                    for tt_ in range(4):
                        self.finish_A(merged[:, tt_, :], ("merged", tt_), mT4[tt_], f"mT4_{tt_}")
                        pendingB.append((tt_, tg * 4 + tt_))
            while pendingB:
                tt2, t2 = pendingB.pop(0)
                self.finish_B(mT4[tt2], f"mT4_{tt2}", wo, "wo", src, dst, row0 + t2 * 128, t2)

    def finish_A(self, m_ap, mkey, mT, mTk):
        ps = self.ps
        BK = self.BK
        for half in range(2):
            for kk in range(4):
                k = half * 4 + kk
                self.tr(ps[4 + half][:, kk * 128:(kk + 1) * 128], m_ap[:, k * 128:(k + 1) * 128], rd=[mkey], wr=[self.RK(4 + half, kk)])
            self.cp(mT[:, half * 4:(half + 1) * 4, :], ps[4 + half][:, :].rearrange("p (a b) -> p a b", a=4), rd=BK(4 + half), wr=[mTk],
                    eng='act' if half else 'dve')

    def finish_B(self, mT, mTk, wo, wok, src, dst, row, t):
        P = self.P
        ps = self.ps
        BK = self.BK
        yt = self.ytile[t % 2]
        yk = f"ytile{t % 2}"
        xr = self.xin[t % 2]
        xk = f"xin{t % 2}"
        P.dma(xr[:], src[row:row + 128, :], writes=[xk])
        for half in range(2):
            for k in range(8):
                self.mm(ps[6 + half][:, :], mT[:, k, :], wo[:, k, half * 512:(half + 1) * 512], start=(k == 0), stop=(k == 7),
                        rd=[mTk, wok], wr=BK(6 + half))
            self.cp(yt[:, half * 512:(half + 1) * 512], ps[6 + half][:, :], rd=BK(6 + half), wr=[yk], eng='act')
        self.resid_ln_store(yt, yk, xr, xk, dst[row:row + 128, :])

    def finish_tile(self, m_ap, mkey, mT, wo, wok, src, dst, row, t):
        self.finish_A(m_ap, mkey, mT, "mT")
        self.finish_B(mT, "mT", wo, wok, src, dst, row, t)

    def xattn(self, l, s, src, dst):
        P = self.P
        I = self.I
        ps = self.ps
        BK = self.BK
        row0 = s * S
        XT = [("xT", t) for t in range(NT)]
        with ExitStack() as es:
            sb = lambda n, shp, dt=F32: self.sb(es, n, shp, dt)
            self.alloc_io(es, nxin=4)
            wq = sb("xwq", [128, 8, D], BF16)
            wo = sb("xwo", [128, 8, D], BF16)
            wkv = [sb(f"wkv{i}", [128, 8, 512], BF16) for i in range(4)]
            for blk in range(4):
                P.dma(wkv[blk][:], self.wcols(I['xa_wkv'], l, blk * 512, (blk + 1) * 512), writes=[f"wkv{blk}"], q='pool')
            P.dma(wq[:], I['xa_wq'][l].rearrange("(k p) n -> p k n", p=128), writes=["xwq"], q='pool')
            P.dma(wo[:], I['xa_wo'][l].rearrange("(k p) n -> p k n", p=128), writes=["xwo"], q='pool')
            self.load_ln('ln2_g', 'ln2_b', l)
            memT = sb("memT", [128, 8, 256], BF16)
            kT = sb("kT", [128, 8, 256], BF16)
            vaug = sb("xvaug", [128, 2, 4, 257], BF16)
            self.memset(vaug[:, :, :, 256:257], 1.0, wr=["xvaug"])
            for mt in range(2):
                buf = self.xin[mt % 2]
                bk = f"xin{mt % 2}"
                P.dma(buf[:], I['mem'][s * 256 + mt * 128: s * 256 + (mt + 1) * 128, :], writes=[bk])
                for half in range(2):
                    for kk in range(4):
                        k = half * 4 + kk
                        self.tr(ps[half][:, kk * 128:(kk + 1) * 128], buf[:, k * 128:(k + 1) * 128], rd=[bk], wr=[self.RK(half, kk)])
                    self.cp(memT[:, half * 4:(half + 1) * 4, mt * 128:(mt + 1) * 128], ps[half][:, :].rearrange("p (a b) -> p a b", a=4),
                            rd=BK(half), wr=["memT"], eng='act' if half else 'dve')
            for blk in range(4):
                wi = blk
                if blk < 2:
                    for cc_ in range(4):
                        c = blk * 4 + cc_
                        bank = ps[2 + cc_ % 2]
                        for k in range(8):
                            self.mm(bank[:, 0:256], wkv[wi][:, k, cc_ * 128:(cc_ + 1) * 128], memT[:, k, :], start=(k == 0), stop=(k == 7),
                                    rd=[f"wkv{wi}", "memT"], wr=BK(2 + cc_ % 2))
                        self.cp(kT[:, c, :], bank[:, 0:256], rd=BK(2 + cc_ % 2), wr=["kT"], eng='act' if cc_ % 2 else 'dve')
                else:
                    vb = blk - 2
                    for mc in range(2):
                        bank = ps[2 + mc]
                        for k in range(8):
                            self.mm(bank[:, :], memT[:, k, mc * 128:(mc + 1) * 128], wkv[wi][:, k, :], start=(k == 0), stop=(k == 7),
                                    rd=[f"wkv{wi}", "memT"], wr=BK(2 + mc))
                        self.cp(vaug[:, mc, 2 * vb:2 * vb + 2, 0:256], bank[:, :].rearrange("p (a b) -> p a b", a=2), rd=BK(2 + mc),
                                wr=["xvaug"], eng='act' if mc else 'dve')
            self.build_xT(src, row0)
            qT = [sb(f"xqT{i}", [128, 8, 512], BF16) for i in range(2)]
            pt = [sb(f"xpt{i}", [128, 2, 512], BF16) for i in range(2)]
            xo = sb("xo", [128, 4, D])
            rinv = [sb(f"xrinv{i}", [128, 1]) for i in range(2)]
            oTt = sb("oTt", [128, 8, 128], BF16)
            pc = 0
            rc = 0
            def qproj_chunk(tg, c):
                q_ = qT[tg % 2]
                qk = f"xqT{tg % 2}"
                bank = ps[c % 2]
                for k in range(8):
                    self.mm(bank[:, :], wq[:, k, c * 128:(c + 1) * 128], self.xT[:, k, tg * 512:(tg + 1) * 512], start=(k == 0), stop=(k == 7),
                            rd=["xwq"] + XT[tg * 4:(tg + 1) * 4], wr=BK(c % 2))
                self.cp(q_[:, c, :], bank[:, :], rd=BK(c % 2), wr=[(qk, c)], eng='act' if c % 2 else 'dve')
            for c in range(8):
                qproj_chunk(0, c)
            for tg in range(4):
                q_ = qT[tg % 2]
                qk = f"xqT{tg % 2}"
                for h in range(4):
                    p_ = pt[pc % 2]
                    pk_ = f"xpt{pc % 2}"
                    pc += 1
                    for mc in range(2):
                        bank = ps[2 + mc]
                        for dc in range(2):
                            self.mm(bank[:, :], kT[:, 2 * h + dc, mc * 128:(mc + 1) * 128], q_[:, 2 * h + dc, :], start=(dc == 0), stop=(dc == 1),
                                    rd=["kT", (qk, 2 * h + dc)], wr=BK(2 + mc))
                        self.act(p_[:, mc, :], bank[:, :], AF.Exp, scale=1.0 / 16.0, rd=BK(2 + mc), wr=[pk_])
                    if tg + 1 < 4:
                        qproj_chunk(tg + 1, 2 * h)
                        qproj_chunk(tg + 1, 2 * h + 1)
                    for tt_ in range(4):
                        ob = ps[4 + tt_ % 2]
                        obk = BK(4 + tt_ % 2)
                        for mc in range(2):
                            self.mm(ob[:, 0:257], p_[:, mc, tt_ * 128:(tt_ + 1) * 128], vaug[:, mc, h, :], start=(mc == 0), stop=(mc == 1),
                                    rd=[pk_, "xvaug"], wr=obk)
                        ri = rinv[rc % 2]
                        rik = f"xrinv{rc % 2}"
                        rc += 1
                        P.op('dve', lambda e, ri=ri, ob=ob: e.reciprocal(out=ri[:], in_=ob[:, 256:257]), obk, [rik])
                        self.ts(xo[:, tt_, h * 256:(h + 1) * 256], ob[:, 0:256], ri[:, 0:1], None, ALU.mult, rd=list(obk) + [rik],
                                wr=[("xo", tt_)])
                for tt_ in range(4):
                    t = tg * 4 + tt_
                    self.finish_tile(xo[:, tt_, :], ("xo", tt_), oTt, wo, "xwo", src, dst, row0 + t * 128, t)

    def moe(self, l, s, src, dst):
        P = self.P
        I = self.I
        ps = self.ps
        BK = self.BK
        row0 = s * S
        XT = [("xT", t) for t in range(NT)]
        with ExitStack() as es:
            sb = lambda n, shp, dt=F32: self.sb(es, n, shp, dt)
            self.alloc_io(es, ytile=False)
            wr_ = sb("mwr", [128, 8, 36])
            P.dma(wr_[:], I['moe_wr_r'][l].rearrange("p (k n) -> p k n", k=8), writes=["gate_w"])
            br = sb("mbr", [128, 36])
            P.dma(br[:], self.bcast(I['moe_br'][l, :]), writes=["mbr"])
            self.load_ln('ln3_g', 'ln3_b', l)
            rl = sb("rl", [128, NT, 36])
            yacc = sb("yacc", [128, NT, D])
            with ExitStack() as e0:
                xTf = self.sb(e0, "xTf", [128, 8, 128], F32)
                self.build_xT(src, row0, wr_, rl, 36, xTf)
                P.barrier()
            G = sb("G", [128, NT, 4])
            gmax = sb("gmax", [128, NT, 1])
            gsum = sb("gsum", [128, NT, 1])
            goh = sb("goh", [128, NT, 4])
            EL = sb("EL", [128, NT, 32])
            m1 = sb("m1", [128, NT, 1])
            m2 = sb("m2", [128, NT, 1])
            oh1 = sb("oh1", [128, NT, 32])
            oh2 = sb("oh2", [128, NT, 32])
            EL2 = sb("EL2", [128, NT, 32])
            wa = sb("wa", [128, NT, 1])
            wb = sb("wb", [128, NT, 1])
            Wc = sb("Wc", [128, NT, 32])
            for t in range(NT):
                self.tt(rl[:, t, :], rl[:, t, :], br[:], ALU.add, rd=["graw", "mbr"], wr=["graw"])
            self.cp(G[:], rl[:, :, 0:4], rd=["graw"], wr=["G"])
            self.cp(EL[:], rl[:, :, 4:36], rd=["graw"], wr=["EL"], eng='act')
            P.op('dve', lambda e: e.tensor_reduce(out=gmax[:], in_=G[:], axis=mybir.AxisListType.X, op=ALU.max), ["G"], ["gmax"])
            for t in range(NT):
                self.ts(goh[:, t, :], G[:, t, :], gmax[:, t, :], None, ALU.is_equal, rd=["G", "gmax"], wr=["goh"])
                self.ts(G[:, t, :], G[:, t, :], gmax[:, t, :], None, ALU.subtract, rd=["G", "gmax"], wr=["G"])
            self.act(G[:], G[:], AF.Exp, rd=["G"], wr=["G"])
            P.op('dve', lambda e: e.tensor_reduce(out=gsum[:], in_=G[:], axis=mybir.AxisListType.X, op=ALU.add), ["G"], ["gsum"])
            P.op('dve', lambda e: e.reciprocal(out=gsum[:], in_=gsum[:]), ["gsum"], ["gsum"])
            self.ts(goh[:], goh[:], -1.0, 30000.0, ALU.add, ALU.mult, rd=["goh"], wr=["goh"])
            for t in range(NT):
                for g in range(4):
                    self.ts(EL[:, t, g * 8:(g + 1) * 8], EL[:, t, g * 8:(g + 1) * 8], goh[:, t, g:g + 1], None, ALU.add,
                            rd=["EL", "goh"], wr=["EL"], eng='pool' if g % 2 else 'dve')
            P.op('dve', lambda e: e.tensor_reduce(out=m1[:], in_=EL[:], axis=mybir.AxisListType.X, op=ALU.max), ["EL"], ["m1"])
            for t in range(NT):
                self.ts(oh1[:, t, :], EL[:, t, :], m1[:, t, :], None, ALU.is_equal, rd=["EL", "m1"], wr=["oh1"])
            self.stt(EL2[:], oh1[:], -30000.0, EL[:], ALU.mult, ALU.add, rd=["oh1", "EL"], wr=["EL2"])
            P.op('dve', lambda e: e.tensor_reduce(out=m2[:], in_=EL2[:], axis=mybir.AxisListType.X, op=ALU.max), ["EL2"], ["m2"])
            for t in range(NT):
                self.ts(oh2[:, t, :], EL2[:, t, :], m2[:, t, :], None, ALU.is_equal, rd=["EL2", "m2"], wr=["oh2"])
            self.tt(m2[:], m2[:], m1[:], ALU.subtract, rd=["m1", "m2"], wr=["m2"])
            self.act(m2[:], m2[:], AF.Exp, rd=["m2"], wr=["m2"])
            self.ts(m1[:], m2[:], 1.0, None, ALU.add, rd=["m2"], wr=["m1"])
            P.op('dve', lambda e: e.reciprocal(out=m1[:], in_=m1[:]), ["m1"], ["m1"])
            self.tt(wa[:], m1[:], gsum[:], ALU.mult, rd=["m1", "gsum"], wr=["wa"])
            self.tt(wb[:], wa[:], m2[:], ALU.mult, rd=["wa", "m2"], wr=["wb"])
            for t in range(NT):
                self.ts(Wc[:, t, :], oh1[:, t, :], wa[:, t, :], None, ALU.mult, rd=["oh1", "wa"], wr=["Wc"])
                self.stt(Wc[:, t, :], oh2[:, t, :], wb[:, t, :], Wc[:, t, :], ALU.mult, ALU.add, rd=["oh2", "wb", "Wc"], wr=["Wc"])
            wg = [sb(f"wg{i}", [128, 8, 256], BF16) for i in range(2)]
            wu = [sb(f"wu{i}", [128, 8, 256], BF16) for i in range(2)]
            wd = [sb(f"wd{i}", [128, 2, D], BF16) for i in range(2)]
            sg = [sb(f"sg{i}", [128, 512]) for i in range(2)]
            hT = [sb(f"hT{i}", [128, 2, 512], BF16) for i in range(2)]

            def load_e(e):
                i = e % 2
                P.dma(wg[i][:], I['moe_w_gate'][l, e].rearrange("(k p) n -> p k n", p=128), writes=[f"wg{i}"], q='pool')
                P.dma(wu[i][:], I['moe_w_up'][l, e].rearrange("(k p) n -> p k n", p=128), writes=[f"wu{i}"], q='pool')
                P.dma(wd[i][:], I['moe_w_down'][l, e].rearrange("(c p) n -> p c n", p=128), writes=[f"wd{i}"], q='pool')
            load_e(0)
            hc = 0
            pending = None

            ytmp = [sb(f"ytmp{i}", [128, 512]) for i in range(2)]
            ycnt = [0]

            def make_y(e, tg, h_, hk, wi):
                def emit_y(tiles):
                    for tt_ in tiles:
                        t = tg * 4 + tt_
                        for half in range(2):
                            bi = 4 + (2 * tt_ + half) % 4
                            bank = ps[bi]
                            bkk = BK(bi)
                            for f in range(2):
                                self.mm(bank[:, :], h_[:, f, tt_ * 128:(tt_ + 1) * 128], wd[wi][:, f, half * 512:(half + 1) * 512],
                                        start=(f == 0), stop=(f == 1), rd=[hk, f"wd{wi}"], wr=bkk)
                            ysl = yacc[:, t, half * 512:(half + 1) * 512]
                            wcol = Wc[:, t, e:e + 1]
                            yk = ("yacc", t, half)
                            if e == 0:
                                self.ts(ysl, bank[:, :], wcol, None, ALU.mult, rd=list(bkk) + ["Wc"], wr=[yk])
                            elif half == 0:
                                self.stt(ysl, bank[:, :], wcol, ysl, ALU.mult, ALU.add, rd=list(bkk) + ["Wc", yk], wr=[yk])
                            else:
                                yt_ = ytmp[ycnt[0] % 2]
                                ytk = f"ytmp{ycnt[0] % 2}"
                                ycnt[0] += 1
                                self.act(yt_[:], bank[:, :], AF.Copy, scale=wcol, rd=list(bkk) + ["Wc"], wr=[ytk])
                                self.tt(ysl, ysl, yt_[:], ALU.add, rd=[ytk, yk], wr=[yk], eng='pool')
                return emit_y
            for e in range(32):
                wi = e % 2
                for tg in range(4):
                    ts_ = slice(tg * 512, (tg + 1) * 512)
                    h_ = hT[hc % 2]
                    hk = f"hT{hc % 2}"
                    hc += 1
                    for f in range(2):
                        for k in range(8):
                            self.mm(ps[f][:, :], wg[wi][:, k, f * 128:(f + 1) * 128], self.xT[:, k, ts_], start=(k == 0), stop=(k == 7),
                                    rd=[f"wg{wi}"] + XT[tg * 4:(tg + 1) * 4], wr=BK(f))
                        for k in range(8):
                            self.mm(ps[2 + f][:, :], wu[wi][:, k, f * 128:(f + 1) * 128], self.xT[:, k, ts_], start=(k == 0), stop=(k == 7),
                                    rd=[f"wu{wi}"] + XT[tg * 4:(tg + 1) * 4], wr=BK(2 + f))
                        s_ = sg[f]
                        sk = f"sg{f}"
                        self.act(s_[:], ps[f][:, :], AF.Silu, rd=BK(f), wr=[sk])
                        self.tt(h_[:, f, :], s_[:], ps[2 + f][:, :], ALU.mult, rd=[sk] + list(BK(2 + f)), wr=[hk])
                        if pending is not None:
                            pending([0, 1] if f == 0 else [2, 3])
                    if tg == 0 and e + 1 < 32 and not _os.environ.get('MOE_NOLOAD'):
                        load_e(e + 1)
                    pending = make_y(e, tg, h_, hk, wi)
            if pending is not None:
                pending([0, 1, 2, 3])
            for t in range(NT):
                xr = self.xin[t % 2]
                xk = f"xin{t % 2}"
                P.dma(xr[:], src[row0 + t * 128: row0 + (t + 1) * 128, :], writes=[xk])
                self.resid_ln_store_keyed(yacc[:, t, :], [("yacc", t, 0), ("yacc", t, 1)], xr, xk, dst[row0 + t * 128: row0 + (t + 1) * 128, :], t)

    def resid_ln_store_keyed(self, yt_ap, ykeys, xres, xkey, dst_rows, t):
        P = self.P
        yk = ykeys
        self.stt(yt_ap, xres[:], ALPHA, yt_ap, ALU.mult, ALU.add, rd=list(yk) + [xkey], wr=yk)
        st, mv, lr = self.lnst, self.lnmv, self.lnr
        for c in range(2):
            P.op('dve', lambda e, c=c: e.bn_stats(out=st[:, c, :], in_=yt_ap[:, c * 512:(c + 1) * 512]), yk, ["lnst"])
        P.op('dve', lambda e: e.bn_aggr(out=mv[:], in_=st[:].rearrange("p a b -> p (a b)")), ["lnst"], ["lnmv"])
        self.act(lr[:], mv[:, 1:2], AF.Ln, bias=1e-5, rd=["lnmv"], wr=["lnr"])
        self.act(lr[:], lr[:], AF.Exp, scale=-0.5, rd=["lnr"], wr=["lnr"])
        self.ts(yt_ap, yt_ap, mv[:, 0:1], lr[:, 0:1], ALU.subtract, ALU.mult, rd=list(yk) + ["lnmv", "lnr"], wr=yk)
        self.tt(yt_ap, yt_ap, self.lng[:], ALU.mult, rd=list(yk) + ["lng"], wr=yk, eng='pool')
        self.tt(yt_ap, yt_ap, self.lnb[:], ALU.add, rd=list(yk) + ["lnb"], wr=yk, eng='pool')
        P.dma(dst_rows, yt_ap, reads=yk, writes=["dram_store"], key=f"st_y{t % 4}")


_CACHE = {}


def _consts():
    i = np.arange(128)
    c = {}
    c['c_ident'] = np.eye(128, dtype=np.float32)
    c['c_tri'] = (i[:, None] <= i[None, :]).astype(np.float32)
    c['c_ones'] = np.ones((128, 128), np.float32)
    c['c_mneg_sl'] = np.where(i[None, :] < i[:, None], 0.0, NEG).astype(np.float32)
    c['c_mneg_iu'] = np.where(i[:, None] <= i[None, :], 0.0, NEG).astype(np.float32)
    c['c_causal'] = (i[:, None] <= i[None, :]).astype(np.float32)
    sel = np.zeros((32, 32, 128), np.float32)
    for e in range(32):
        sel[e, e, :] = 1.0
    c['c_sel'] = sel.reshape(32, 32 * 128)
    return c


def make_in_maps(inputs, n_cores=N_CORES):
    f = lambda a: np.ascontiguousarray(np.asarray(a, dtype=np.float32))
    shared = {}
    for k in ['w_in', 'gdn_norm_w', 'gla_w_gate2', 'gla_b_gate', 'gla_norm_w',
              'p_gdn', 'p_fox', 'p_gla', 'b_merge', 'w_out', 'ln1_g', 'ln1_b', 'xa_wq', 'xa_wkv', 'xa_wo', 'ln2_g', 'ln2_b',
              'moe_w_gate', 'moe_w_up', 'moe_w_down', 'ln3_g', 'ln3_b']:
        shared[k] = f(inputs[k])
    cw = np.transpose(np.asarray(inputs['gdn_conv_w']), (0, 2, 1))
    shared['conv_wT'] = f(cw.reshape(DEPTH, 12, 128, 4).transpose(0, 2, 1, 3).reshape(DEPTH, 128, 48))
    w_in = np.asarray(inputs['w_in'])
    gcols = list(range(OFF['gdn_b'], OFF['gdn_b'] + 8)) + list(range(OFF['fox_f'], OFF['fox_f'] + 8)) + list(range(OFF['gla_lr'], OFF['gla_lr'] + 16))
    wg_ = w_in[:, :, gcols]
    shared['w_gate_r'] = f(wg_.reshape(DEPTH, 8, 128, 32).transpose(0, 2, 1, 3).reshape(DEPTH, 128, 256))
    wr_ = np.concatenate([np.asarray(inputs['moe_w_group']), np.asarray(inputs['moe_w_expert'])], axis=-1)
    shared['moe_wr_r'] = f(wr_.reshape(DEPTH, 8, 128, 36).transpose(0, 2, 1, 3).reshape(DEPTH, 128, 288))
    shared['alog_r'] = f(np.tile(np.asarray(inputs['gdn_a_log']), (1, NT)))
    shared['dtb_r'] = f(np.tile(np.asarray(inputs['gdn_dt_bias']), (1, NT)))
    shared['fb_r'] = f(np.tile(np.asarray(inputs['fox_f_bias']), (1, NT)))
    shared['moe_br'] = f(np.concatenate([np.asarray(inputs['moe_b_group']), np.asarray(inputs['moe_b_expert'])], axis=-1))
    shared.update(_consts())
    x = f(inputs['x'])
    mem = f(inputs['mem'])
    maps = []
    for c in range(n_cores):
        m = dict(shared)
        m['x'] = x[c * NSEQ:(c + 1) * NSEQ].reshape(NTOK, D)
        m['mem'] = mem[c * NSEQ:(c + 1) * NSEQ].reshape(NSEQ * 256, D)
        maps.append(m)
    return maps


def kernel(**inputs):
    if 'nc' not in _CACHE:
        _CACHE['nc'] = Builder().build()
    nc = _CACHE['nc']
    maps = make_in_maps(inputs)
    res = run_bass_kernel_spmd(nc, maps, core_ids=list(range(N_CORES)))
    out = np.concatenate([r["out"].reshape(NSEQ, S, D) for r in res.results], axis=0)
    return out.astype(np.float32)
```

```python
import numpy as np
import concourse.bass as bass
import concourse.mybir as mybir
from concourse.bass_utils import run_bass_kernel_spmd
from contextlib import ExitStack

F32 = mybir.dt.float32
BF16 = mybir.dt.bfloat16
AF = mybir.ActivationFunctionType
ALU = mybir.AluOpType

import os as _os
SAME_ENG_SYNC = _os.environ.get('SES', '1') == '1'
N_CORES = 8
DEPTH = 2
D = 1024
S = 2048
NT = 16
NSEQ = 2
NTOK = NSEQ * S
ALPHA = float((2 * DEPTH) ** 0.25)
OFF = dict(gdn_q=0, gdn_k=512, gdn_v=1024, gdn_b=1536, gdn_a=1540, gdn_z=1544, fox_q=2056, fox_k=2568,
           fox_v=3080, fox_f=3592, gla_q=3600, gla_k=3856, gla_v=4112, gla_r=4624, gla_lr=5136, merge=5152)
N_IN = 8224
NEG = -30000.0


class Prog:
    ENG = ('pe', 'act', 'dve', 'pool', 'sp')

    def __init__(self, nc, es):
        self.nc = nc
        self.es = es
        self.q = {e: [] for e in self.ENG}
        self.sems = {}
        self.cnt = {}
        self.seen = {e: {} for e in self.ENG}
        self.wr = {}
        self.rd = {}
        for e in ('pe', 'act', 'dve', 'pool'):
            self._sem('E_' + e)
        self.n_ops = 0

    def _sem(self, name):
        if name not in self.sems:
            self.sems[name] = self.es.enter_context(self.nc.semaphore(name))
            self.cnt[name] = 0
        return self.sems[name]

    @staticmethod
    def _norm(keys):
        out = []
        for k in keys:
            if isinstance(k, str) and len(k) == 4 and k[:2] == 'ps' and k[3] in 'bcd':
                k = k[:3]
            if k not in out:
                out.append(k)
        return tuple(out)

    def _collect(self, eng, reads, writes):
        need = {}

        def add(tok):
            for s, v in tok.items():
                if v > need.get(s, 0):
                    need[s] = v
        for r in reads:
            add(self.wr.get(r, {}))
            if isinstance(r, str) and r[:2] == 'ps' and len(r) == 3:
                add({s: v for s, v in self.rd.get(r, {}).items() if s != 'E_' + eng})
        for w in writes:
            add(self.wr.get(w, {}))
            add(self.rd.get(w, {}))
        waits = []
        own = 'E_' + eng
        for s, v in need.items():
            if s == own and (eng == 'pe' or not SAME_ENG_SYNC):
                continue
            if self.seen[eng].get(s, 0) >= v:
                continue
            self.seen[eng][s] = v
            waits.append((s, v))
        return waits

    def _commit(self, reads, writes, tok):
        for r in reads:
            d = self.rd.setdefault(r, {})
            for s, v in tok.items():
                if v > d.get(s, 0):
                    d[s] = v
        for w in writes:
            self.wr[w] = dict(tok)
            self.rd[w] = {}

    def op(self, eng, fn, reads=(), writes=(), inc=True):
        reads = self._norm(reads)
        writes = self._norm(writes)
        waits = self._collect(eng, reads, writes)
        s = 'E_' + eng
        if inc:
            self.cnt[s] += 1
            tok = {s: self.cnt[s]}
            self.q[eng].append((waits, fn, (s, 1)))
        else:
            tok = {s: self.cnt[s] + 1}
            self.q[eng].append((waits, fn, None))
        self._commit(reads, writes, tok)
        self.n_ops += 1

    def dma(self, out, in_, reads=(), writes=(), q='sp', key=None, **kw):
        reads = tuple(reads)
        writes = tuple(writes)
        waits = self._collect(q, reads, writes)
        if key is None:
            key = writes[0]
        s = ('W_' if q == 'pool' else 'D_') + str(key)
        self._sem(s)
        self.cnt[s] += 16
        tok = {s: self.cnt[s]}
        self.q[q].append((waits, lambda e: e.dma_start(out=out, in_=in_, **kw), (s, 16)))
        self._commit(reads, writes, tok)
        self.n_ops += 1

    def barrier(self):
        if _os.environ.get('NOBAR'):
            return
        for e in self.ENG:
            if e == _os.environ.get('BARSKIP'):
                continue
            waits = []
            for s, c in self.cnt.items():
                if c > 0 and self.seen[e].get(s, 0) < c:
                    if s == 'E_' + e:
                        continue
                    self.seen[e][s] = c
                    waits.append((s, c))
            if waits:
                self.q[e].append((waits, None, None))

    def finish(self):
        waits = []
        for s, c in self.cnt.items():
            if c > 0 and self.seen['sp'].get(s, 0) < c:
                waits.append((s, c))
        self.q['sp'].append((waits, None, None))

    def emit(self):
        nc = self.nc

        def run(name, e):
            for waits, fn, inc in self.q[name]:
                for s, v in waits:
                    e.wait_ge(self.sems[s], v)
                if fn is not None:
                    ins = fn(e)
                    if inc is not None:
                        ins.then_inc(self.sems[inc[0]], inc[1])
        with nc.Block() as block:
            @block.tensor
            def _(e):
                run('pe', e)

            @block.scalar
            def _(e):
                run('act', e)

            @block.vector
            def _(e):
                run('dve', e)

            @block.gpsimd
            def _(e):
                run('pool', e)

            @block.sync
            def _(e):
                run('sp', e)


class Builder:
    def __init__(self, debug=False, stages=('mix', 'xa', 'moe'), layers=(0, 1), nseq=NSEQ, sub=('gdn', 'fox', 'gla', 'merge')):
        self.debug = debug
        self.sub = sub
        self.stages = stages
        self.layers = layers
        self.nseq = nseq
        self.nc = bass.Bass("TRN2", target_bir_lowering=False)
        self.uid = 0
        self.names = {}

    def din(self, name, shape, dt=F32):
        return self.nc.dram_tensor(name, list(shape), dt, kind="ExternalInput").ap()

    def sb(self, es, name, shape, dt):
        self.uid += 1
        self.names[name] = f"{name}_{self.uid}"
        return es.enter_context(self.nc.sbuf_tensor(f"{name}_{self.uid}", list(shape), dt))

    def mm(self, out, lhsT, rhs, start=True, stop=True, rd=(), wr=(), skip=False, inc=None):
        kw = dict(start=start, stop=stop)
        if skip:
            kw['skip_group_check'] = True
        if inc is None:
            inc = stop or skip
        self.P.op('pe', lambda e: e.matmul(out, lhsT=lhsT, rhs=rhs, **kw), rd, wr, inc=inc)

    def tr(self, out, in_, rd=(), wr=()):
        idt = self.ident
        self.P.op('pe', lambda e: e.transpose(out=out, in_=in_, identity=idt[:]), tuple(rd) + ("const",), wr)

    def act(self, out, in_, func, bias=None, scale=None, rd=(), wr=(), accum=None):
        kw = {}
        if bias is not None:
            kw['bias'] = bias
        if scale is not None:
            kw['scale'] = scale
        if accum is not None:
            kw['accum_out'] = accum
        self.P.op('act', lambda e: e.activation(out=out, in_=in_, func=func, **kw), rd, wr)

    def ts(self, out, in0, s1, s2, op0, op1=None, rd=(), wr=(), eng='dve', accum=None):
        kw = {}
        if op1 is not None:
            kw['op1'] = op1
        if accum is not None:
            kw['accum_out'] = accum
        self.P.op(eng, lambda e: e.tensor_scalar(out=out, in0=in0, scalar1=s1, scalar2=s2, op0=op0, **kw), rd, wr)

    def tt(self, out, in0, in1, op, rd=(), wr=(), eng='dve'):
        self.P.op(eng, lambda e: e.tensor_tensor(out=out, in0=in0, in1=in1, op=op), rd, wr)

    def stt(self, out, in0, scalar, in1, op0, op1, rd=(), wr=()):
        self.P.op('dve', lambda e: e.scalar_tensor_tensor(out=out, in0=in0, scalar=scalar, in1=in1, op0=op0, op1=op1), rd, wr)

    def cp(self, out, in_, rd=(), wr=(), eng='dve'):
        if eng == 'act':
            self.P.op('act', lambda e: e.activation(out=out, in_=in_, func=AF.Copy), rd, wr)
        else:
            self.P.op(eng, lambda e: e.tensor_copy(out=out, in_=in_), rd, wr)

    def memset(self, ap, val, wr=(), eng='dve'):
        self.P.op(eng, lambda e: e.memset(ap, val), (), wr)

    def wcols(self, w_ap, l, c0, c1):
        return w_ap[l, :, c0:c1].rearrange("(k p) n -> p k n", p=128)

    def bcast(self, ap1d):
        return ap1d.partition_broadcast(128)

    @staticmethod
    def RK(i, r):
        return f"ps{i}" + ["", "b", "c", "d"][r]

    @staticmethod
    def BK(i):
        return (f"ps{i}", f"ps{i}b", f"ps{i}c", f"ps{i}d")

    def build(self):
        nc = self.nc
        dbg = self.debug
        I = {}
        I['x'] = self.din("x", [NTOK, D])
        I['mem'] = self.din("mem", [NSEQ * 256, D])
        I['w_in'] = self.din("w_in", [DEPTH, D, N_IN])
        I['conv_wT'] = self.din("conv_wT", [DEPTH, 128, 48])
        I['w_gate_r'] = self.din("w_gate_r", [DEPTH, 128, 256])
        I['moe_wr_r'] = self.din("moe_wr_r", [DEPTH, 128, 288])
        I['alog_r'] = self.din("alog_r", [DEPTH, 64])
        I['dtb_r'] = self.din("dtb_r", [DEPTH, 64])
        I['fb_r'] = self.din("fb_r", [DEPTH, 128])
        for n, shp in [('gdn_norm_w', [DEPTH, 128]),
                       ('gla_w_gate2', [DEPTH, 16, 256]), ('gla_b_gate', [DEPTH, 256]),
                       ('gla_norm_w', [DEPTH, 128]), ('p_gdn', [DEPTH, 512, D]), ('p_fox', [DEPTH, 512, D]),
                       ('p_gla', [DEPTH, 512, D]), ('b_merge', [DEPTH, 3072]), ('w_out', [DEPTH, D, D]),
                       ('ln1_g', [DEPTH, D]), ('ln1_b', [DEPTH, D]), ('xa_wq', [DEPTH, D, D]),
                       ('xa_wkv', [DEPTH, D, 2 * D]), ('xa_wo', [DEPTH, D, D]), ('ln2_g', [DEPTH, D]),
                       ('ln2_b', [DEPTH, D]), ('moe_br', [DEPTH, 36]),
                       ('moe_w_gate', [DEPTH, 32, D, 256]), ('moe_w_up', [DEPTH, 32, D, 256]),
                       ('moe_w_down', [DEPTH, 32, 256, D]), ('ln3_g', [DEPTH, D]), ('ln3_b', [DEPTH, D]),
                       ('c_ident', [128, 128]), ('c_tri', [128, 128]), ('c_ones', [128, 128]),
                       ('c_mneg_sl', [128, 128]), ('c_mneg_iu', [128, 128]), ('c_causal', [128, 128]),
                       ('c_sel', [32, 32 * 128])]:
            I[n] = self.din(n, shp)
        self.I = I
        kind = "ExternalOutput" if dbg else "Internal"
        self.out = nc.dram_tensor("out", [NTOK, D], F32, kind="ExternalOutput").ap()
        self.scr = [nc.dram_tensor(f"scr{i}", [NTOK, D], F32, kind=kind).ap() for i in range(3)]
        with ExitStack() as es:
            self.P = Prog(nc, es)
            self.ps = [es.enter_context(nc.psum_tensor(f"ps{i}", [128, 512], F32)) for i in range(8)]
            self.ident = self.sb(es, "ident", [128, 128], F32)
            self.identb = self.sb(es, "identb", [128, 128], BF16)
            self.tri = self.sb(es, "tri", [128, 128], F32)
            self.ones = self.sb(es, "ones", [128, 128], F32)
            self.mneg_sl = self.sb(es, "mneg_sl", [128, 128], BF16)
            self.mneg_iu = self.sb(es, "mneg_iu", [128, 128], BF16)
            self.causal = self.sb(es, "causal", [128, 128], F32)
            self.causalb = self.sb(es, "causalb", [128, 128], BF16)
            P = self.P
            P.dma(self.ident[:], I['c_ident'], writes=["const"], key="const")
            P.dma(self.tri[:], I['c_tri'], writes=["const"], key="const")
            P.dma(self.ones[:], I['c_ones'], writes=["const"], key="const")
            P.dma(self.causal[:], I['c_causal'], writes=["const"], key="const")
            if not _os.environ.get('NOPOOLC'):
                P.dma(self.identb[:], I['c_ident'], writes=["const"], key="const", q='pool')
                P.dma(self.mneg_sl[:], I['c_mneg_sl'], writes=["const"], key="const", q='pool')
                P.dma(self.mneg_iu[:], I['c_mneg_iu'], writes=["const"], key="const", q='pool')
                P.dma(self.causalb[:], I['c_causal'], writes=["const"], key="const", q='pool')
            self.xT = self.sb(es, "xT", [128, 8, S], BF16)

            for l in self.layers:
                src = I['x'] if l == 0 else self.scr[2]
                dst = self.out if l == DEPTH - 1 else self.scr[2]
                if 'mix' in self.stages:
                    for s in range(self.nseq):
                        self.mixer(l, s, src, self.scr[0])
                if 'xa' in self.stages:
                    for s in range(self.nseq):
                        self.xattn(l, s, self.scr[0] if 'mix' in self.stages else I['x'], self.scr[1])
                        P.barrier()
                if 'moe' in self.stages:
                    for s in range(self.nseq):
                        self.moe(l, s, self.scr[1] if 'xa' in self.stages else I['x'], dst)
                        P.barrier()
            P.finish()
            P.emit()
        return nc

    def alloc_io(self, es, ln=True, ytile=True, nxin=2):
        self.xin = [self.sb(es, f"xin{i}", [128, D], F32) for i in range(nxin)]
        if ln:
            self.lng = self.sb(es, "lng", [128, D], F32)
            self.lnb = self.sb(es, "lnb", [128, D], F32)
            self.lnst = self.sb(es, "lnst", [128, 2, 6], F32)
            self.lnmv = self.sb(es, "lnmv", [128, 2], F32)
            self.lnr = self.sb(es, "lnr", [128, 1], F32)
        if ytile:
            self.ytile = [self.sb(es, f"ytile{i}", [128, D], F32) for i in range(2)]

    def load_ln(self, gname, bname, l):
        P = self.P
        P.dma(self.lng[:], self.bcast(self.I[gname][l, :]), writes=["lng"])
        P.dma(self.lnb[:], self.bcast(self.I[bname][l, :]), writes=["lnb"])

    def resid_ln_store(self, yt, ykey, xres, xkey, dst_rows):
        P = self.P
        self.stt(yt[:], xres[:], ALPHA, yt[:], ALU.mult, ALU.add, rd=[ykey, xkey], wr=[ykey])
        for c in range(2):
            st = self.lnst
            P.op('dve', lambda e, c=c: e.bn_stats(out=st[:, c, :], in_=yt[:, c * 512:(c + 1) * 512]), [ykey], ["lnst"])
        st, mv, lr = self.lnst, self.lnmv, self.lnr
        P.op('dve', lambda e: e.bn_aggr(out=mv[:], in_=st[:].rearrange("p a b -> p (a b)")), ["lnst"], ["lnmv"])
        self.act(lr[:], mv[:, 1:2], AF.Ln, bias=1e-5, rd=["lnmv"], wr=["lnr"])
        self.act(lr[:], lr[:], AF.Exp, scale=-0.5, rd=["lnr"], wr=["lnr"])
        self.ts(yt[:], yt[:], mv[:, 0:1], lr[:, 0:1], ALU.subtract, ALU.mult, rd=[ykey, "lnmv", "lnr"], wr=[ykey])
        self.tt(yt[:], yt[:], self.lng[:], ALU.mult, rd=[ykey, "lng"], wr=[ykey], eng='pool')
        self.tt(yt[:], yt[:], self.lnb[:], ALU.add, rd=[ykey, "lnb"], wr=[ykey], eng='pool')
        P.dma(dst_rows, yt[:], reads=[ykey], writes=["dram_store"], key="st_" + ykey)

    def build_xT(self, src, row0, gate_w=None, gate_out=None, gate_n=0, xTf=None):
        P = self.P
        ps = self.ps
        import os
        for t in range(int(os.environ.get("NT_X", NT))):
            nx = len(self.xin)
            buf = self.xin[t % nx]
            bk = f"xin{t % nx}"
            P.dma(buf[:], src[row0 + t * 128: row0 + (t + 1) * 128, :], writes=[bk])
            for half in range(2):
                pk = self.BK(half)
                for kk in range(4):
                    k = half * 4 + kk
                    self.tr(ps[half][:, kk * 128:(kk + 1) * 128], buf[:, k * 128:(k + 1) * 128], rd=[bk], wr=[self.RK(half, kk)])
                pview = ps[half][:, :].rearrange("p (a b) -> p a b", a=4)
                self.cp(self.xT[:, half * 4:(half + 1) * 4, t * 128:(t + 1) * 128], pview, rd=pk, wr=[("xT", t)], eng='act')
                if xTf is not None:
                    self.cp(xTf[:, half * 4:(half + 1) * 4, :], pview, rd=pk, wr=["xTf"], eng='dve')
            if gate_w is not None:
                for k in range(8):
                    self.mm(ps[2][:, 0:gate_n], xTf[:, k, :], gate_w[:, k, :], start=(k == 0), stop=(k == 7),
                            rd=["xTf", "gate_w"], wr=["ps2"])
                self.cp(gate_out[:, t, :], ps[2][:, 0:gate_n], rd=["ps2"], wr=["graw"])

    def mixer(self, l, s, src, dst):
        P = self.P
        I = self.I
        ps = self.ps
        row0 = s * S
        XT = [("xT", t) for t in range(NT)]
        with ExitStack() as es:
            oT = self.sb(es, "oT", [128, 12, S], BF16)
            graw = self.sb(es, "graw", [128, NT, 32], F32)
            with ExitStack() as e0:
                self.alloc_io(e0, ln=False, ytile=False, nxin=4)
                xTf = self.sb(e0, "xTf", [128, 8, 128], F32)
                gate_w = self.sb(e0, "gate_w", [128, 8, 32], F32)
                P.dma(gate_w[:], I['w_gate_r'][l].rearrange("p (k n) -> p k n", k=8), writes=["gate_w"])
                if _os.environ.get('NOGATE'):
                    self.build_xT(src, row0)
                else:
                    self.build_xT(src, row0, gate_w, graw, 32, xTf)
                P.barrier()
            sub = self.sub
            if 'gdn' in sub:
                self.gdn(l, graw, oT, XT)
                P.barrier()
            if 'fox' in sub:
                self.fox(l, graw, oT, XT)
                P.barrier()
            if 'gla' in sub:
                self.gla(l, graw, oT, XT)
                P.barrier()
            if 'merge' in sub:
                self.merge(l, s, src, dst, oT, XT)
                P.barrier()

    def gdn(self, l, graw, oT, XT):
        P = self.P
        I = self.I
        ps = self.ps
        with ExitStack() as es:
            sb = lambda n, shp, dt=F32: self.sb(es, n, shp, dt)
            cw = sb("cw", [128, 12, 4])
            P.dma(cw[:], I['conv_wT'][l].rearrange("p (b i) -> p b i", b=12), writes=["cw"])
            nw = sb("nw", [128, 128])
            P.dma(nw[:], self.bcast(I['gdn_norm_w'][l, :]), writes=["nw"])
            alog = sb("alog", [128, NT, 4])
            dtb = sb("dtb", [128, NT, 4])
            P.dma(alog[:].rearrange("p a b -> p (a b)"), self.bcast(I['alog_r'][l, :]), writes=["alog"])
            P.dma(dtb[:].rearrange("p a b -> p (a b)"), self.bcast(I['dtb_r'][l, :]), writes=["dtb"])
            beta = sb("beta", [128, NT, 4])
            gg = sb("gg", [128, NT, 4])
            gc = sb("gc", [128, NT, 4])
            ngc = sb("ngc", [128, NT, 4])
            gtot = sb("gtot", [128, NT, 4])
            eg = sb("eg", [128, NT, 4])
            eqs = sb("eqs", [128, NT, 4])
            edec = sb("edec", [128, NT, 4])
            etot = sb("etot", [128, NT, 4])
            beg = sb("beg", [128, NT, 4])
            self.act(beta[:], graw[:, :, 0:4], AF.Sigmoid, rd=["graw"], wr=["beta"])
            self.tt(gg[:], graw[:, :, 4:8], dtb[:], ALU.add, rd=["graw", "dtb"], wr=["gg"])
            self.act(gg[:], gg[:], AF.Exp, rd=["gg"], wr=["gg"])
            self.act(gg[:], gg[:], AF.Ln, bias=1.0, rd=["gg"], wr=["gg"])
            self.act(alog[:], alog[:], AF.Exp, rd=["alog"], wr=["alog"])
            self.stt(gg[:], gg[:], -1.0, alog[:], ALU.mult, ALU.mult, rd=["gg", "alog"], wr=["gg"])
            for t in range(NT):
                self.mm(ps[2][:, 0:4], self.tri[:], gg[:, t, :], rd=["gg", "const"], wr=["ps2"])
                self.mm(ps[2][:, 4:8], self.ones[:], gg[:, t, :], rd=["gg", "const"], wr=["ps2"])
                self.cp(gc[:, t, :], ps[2][:, 0:4], rd=["ps2"], wr=["gc"])
                self.cp(gtot[:, t, :], ps[2][:, 4:8], rd=["ps2"], wr=["gtot"], eng='act')
            self.ts(ngc[:], gc[:], -1.0, None, ALU.mult, rd=["gc"], wr=["ngc"])
            self.act(eg[:], gc[:], AF.Exp, rd=["gc"], wr=["eg"])
            self.ts(eqs[:], eg[:], 128.0 ** -0.5, None, ALU.mult, rd=["eg"], wr=["eqs"])
            self.tt(edec[:], gtot[:], gc[:], ALU.subtract, rd=["gtot", "gc"], wr=["edec"])
            self.act(edec[:], edec[:], AF.Exp, rd=["edec"], wr=["edec"])
            self.act(etot[:], gtot[:], AF.Exp, rd=["gtot"], wr=["etot"])
            self.tt(beg[:], beta[:], eg[:], ALU.mult, rd=["beta", "eg"], wr=["beg"])
            GATES = ["beta", "gc", "ngc", "eg", "eqs", "edec", "etot", "beg"]
            wqkv = [sb(f"wqkv{i}", [128, 8, 3, 128], BF16) for i in range(2)]
            wz = [sb(f"wz{i}", [128, 8, 128], BF16) for i in range(2)]
            pre = sb("pre", [128, S + 4])
            qkvT = [sb(f"qkvT{j}", [128, S]) for j in range(3)]
            self.memset(pre[:, 0:3], 0.0, wr=["pre"])
            NTS, NHS, NRS = 4, 8, 2
            TS, HS, RS = [], [], []
            for b in range(NTS):
                d = {n: sb(f"T{n}{b}", [128, 128]) for n in ["dgn", "dgp", "ET", "E", "L0", "M0", "La", "Lb", "Ma", "Mb"]}
                d["Y0"] = sb(f"TY0{b}", [128, 256])
                TS.append(d)
            for b in range(NHS):
                d = {n: sb(f"H{n}{b}", [128, 128], BF16) for n in ["attnT", "kdec"]}
                d["wT"] = sb(f"HwT{b}", [128, 128])
                d["Y1"] = sb(f"HY1{b}", [128, 256])
                HS.append(d)
            for b in range(NRS):
                d = {n: sb(f"R{n}{b}", [128, 128]) for n in ["As", "ot"]}
                d["vnew"] = sb(f"Rvnew{b}", [128, 128], BF16)
                d["ss"] = sb(f"Rss{b}", [128, 1])
                RS.append(d)
            Sst = [sb(f"Sst{i}", [128, 128]) for i in range(2)]
            zs_all = sb("zs_all", [128, NT, 128])

            def load_head_w(h):
                i = h % 2
                for j, nm in enumerate(['gdn_q', 'gdn_k', 'gdn_v']):
                    P.dma(wqkv[i][:, :, j, :], self.wcols(I['w_in'], l, OFF[nm] + h * 128, OFF[nm] + (h + 1) * 128),
                          writes=[f"wqkv{i}"], q='pool')
                P.dma(wz[i][:], self.wcols(I['w_in'], l, OFF['gdn_z'] + h * 128, OFF['gdn_z'] + (h + 1) * 128),
                      writes=[f"wz{i}"], q='pool')
            load_head_w(0)
            for h in range(4):
                wi = h % 2
                if h + 1 < 4:
                    load_head_w(h + 1)
                for j in range(3):
                    for tg in range(4):
                        bank = ps[tg % 2]
                        pk = self.BK(tg % 2)
                        for k in range(8):
                            self.mm(bank[:, :], wqkv[wi][:, k, j, :], self.xT[:, k, tg * 512:(tg + 1) * 512],
                                    start=(k == 0), stop=(k == 7), rd=[f"wqkv{wi}"] + XT[tg * 4:(tg + 1) * 4], wr=pk)
                        self.cp(pre[:, 3 + tg * 512: 3 + (tg + 1) * 512], bank[:, :], rd=pk, wr=["pre"], eng='act')
                    dstT = qkvT[j]
                    dk_ = f"qkvT{j}"
                    blk = j * 4 + h
                    self.ts(dstT[:], pre[:, 0:S], cw[:, blk, 0:1], None, ALU.mult, rd=["pre", "cw"], wr=[dk_])
                    for i in range(1, 4):
                        self.stt(dstT[:], pre[:, i:i + S], cw[:, blk, i:i + 1], dstT[:], ALU.mult, ALU.add,
                                 rd=["pre", "cw", dk_], wr=[dk_])
                    self.act(dstT[:], dstT[:], AF.Silu, rd=[dk_], wr=[dk_])
                    if j < 2:
                        sq = pre[:, 4:4 + S]
                        self.act(sq, dstT[:], AF.Square, rd=[dk_], wr=["pre"])
                        for tg in range(4):
                            bank = ps[2 + tg % 2]
                            pk = self.BK(2 + tg % 2)
                            self.mm(bank[:, :], self.ones[:], sq[:, tg * 512:(tg + 1) * 512], rd=["pre", "const"], wr=pk)
                            self.act(sq[:, tg * 512:(tg + 1) * 512], bank[:, :], AF.Ln, bias=1e-6, rd=pk, wr=["pre"])
                        self.act(sq, sq, AF.Exp, scale=-0.5, rd=["pre"], wr=["pre"])
                        self.tt(dstT[:], dstT[:], sq, ALU.mult, rd=[dk_, "pre"], wr=[dk_])
                qT, kT, vT = qkvT
                self.memset(Sst[0][:], 0.0, wr=["Sst0"])
                for t in range(NT):
                    zb = ps[4 + t % 2]
                    for k in range(8):
                        self.mm(zb[:, 0:128], self.xT[:, k, t * 128:(t + 1) * 128], wz[wi][:, k, :], start=(k == 0), stop=(k == 7),
                                rd=[XT[t], f"wz{wi}"], wr=[f"ps{4 + t % 2}"])
                    self.cp(zs_all[:, t, :], zb[:, 0:128], rd=[f"ps{4 + t % 2}"], wr=["zs_all"], eng='dve')
                self.act(zs_all[:], zs_all[:], AF.Silu, rd=["zs_all"], wr=["zs_all"])

                def pre_gen(c, h=h, qT=qT, kT=kT, vT=vT):
                    T = TS[c % NTS]
                    H = HS[c % NHS]
                    tb, hb = c % NTS, c % NHS
                    TK = lambda n: f"T{n}{tb}"
                    HK = lambda n: f"H{n}{hb}"
                    cs = slice(c * 128, (c + 1) * 128)
                    col = lambda tile: tile[:, c, h:h + 1]
                    pA, pB, pC = ps[c % 2], ps[2 + c % 2], ps[4 + c % 2]
                    kA, kB, kC = f"ps{c % 2}", f"ps{2 + c % 2}", f"ps{4 + c % 2}"
                    self.ts(T["dgn"][:], self.ident[:], col(ngc), None, ALU.mult, rd=["const", "ngc"], wr=[TK("dgn")])
                    self.ts(T["dgp"][:], self.ident[:], col(gc), None, ALU.mult, rd=["const", "gc"], wr=[TK("dgp")], eng='pool')
                    yield
                    self.mm(pA[:, 0:128], self.ones[:], T["dgn"][:], start=True, stop=False, rd=["const", TK("dgn")], wr=[kA])
                    self.mm(pA[:, 0:128], self.identb[:], self.mneg_sl[:], start=False, stop=True, rd=["const"], wr=[kA])
                    self.mm(pA[:, 128:256], self.ones[:], T["dgp"][:], start=True, stop=False, rd=["const", TK("dgp")], wr=[kA])
                    self.mm(pA[:, 128:256], self.identb[:], self.mneg_iu[:], start=False, stop=True, rd=["const"], wr=[kA])
                    self.mm(pA[:, 256:384], kT[:, cs], kT[:, cs], rd=["qkvT1"], wr=[kA])
                    self.mm(pA[:, 384:512], kT[:, cs], qT[:, cs], rd=["qkvT0", "qkvT1"], wr=[kA])
                    self.act(T["ET"][:], pA[:, 0:128], AF.Exp, bias=col(gc), rd=[kA, "gc"], wr=[TK("ET")])
                    self.act(T["E"][:], pA[:, 128:256], AF.Exp, bias=col(ngc), rd=[kA, "ngc"], wr=[TK("E")])
                    self.stt(T["L0"][:], pA[:, 256:384], col(beta), T["ET"][:], ALU.mult, ALU.mult, rd=[kA, "beta", TK("ET")], wr=[TK("L0")])
                    self.stt(H["attnT"][:], pA[:, 384:512], 128.0 ** -0.5, T["E"][:], ALU.mult, ALU.mult, rd=[kA, TK("E")], wr=[HK("attnT")])
                    yield
                    self.tr(pB[:, 0:128], T["L0"][:], rd=[TK("L0")], wr=[kB])
                    self.tr(pB[:, 128:256], kT[:, cs], rd=["qkvT1"], wr=[kB])
                    self.tr(pB[:, 256:384], vT[:, cs], rd=["qkvT2"], wr=[kB])
                    self.cp(T["M0"][:], pB[:, 0:128], rd=[kB], wr=[TK("M0")], eng='act')
                    self.act(H["kdec"][:], pB[:, 128:256], AF.Copy, scale=col(edec), rd=[kB, "edec"], wr=[HK("kdec")])
                    Y0, Y1 = T["Y0"], H["Y1"]
                    self.ts(Y0[:, 128:256], pB[:, 128:256], col(beg), None, ALU.mult, rd=[kB, "beg"], wr=[TK("Y0")])
                    self.ts(Y0[:, 0:128], pB[:, 256:384], col(beta), None, ALU.mult, rd=[kB, "beta"], wr=[TK("Y0")])
                    yield
                    Ys = [(Y0, TK("Y0")), (Y1, HK("Y1"))]
                    yi = 0
                    Lc, Mc, Lk, Mk = T["L0"], T["M0"], TK("L0"), TK("M0")
                    nxt = [(T["La"], T["Ma"], TK("La"), TK("Ma")), (T["Lb"], T["Mb"], TK("Lb"), TK("Mb"))]
                    for lev in range(7):
                        (Yc, Yck), (Yn, Ynk) = Ys[yi], Ys[1 - yi]
                        self.mm(pB[:, 0:256], Mc[:], Yc[:], rd=[Mk, Yck], wr=[kB])
                        if lev < 6:
                            Ln_, Mn_, Lnk, Mnk = nxt[lev % 2]
                            self.mm(pC[:, 0:128], Lc[:], Mc[:], rd=[Lk, Mk], wr=[kC])
                            if lev < 5:
                                self.mm(pC[:, 128:256], Mc[:], Lc[:], rd=[Lk, Mk], wr=[kC])
                        self.tt(Yn[:], Yc[:], pB[:, 0:256], ALU.subtract if lev == 0 else ALU.add, rd=[Yck, kB], wr=[Ynk])
                        if lev < 6:
                            self.cp(Mn_[:], pC[:, 0:128], rd=[kC], wr=[Mnk], eng='act')
                            if lev < 5:
                                self.cp(Ln_[:], pC[:, 128:256], rd=[kC], wr=[Lnk], eng='act')
                            Lc, Mc, Lk, Mk = Ln_, Mn_, Lnk, Mnk
                        yi = 1 - yi
                        yield
                    assert yi == 1
                    self.tr(pC[:, 256:384], Y1[:, 128:256], rd=[HK("Y1")], wr=[kC])
                    self.cp(H["wT"][:], pC[:, 256:384], rd=[kC], wr=[HK("wT")], eng='act')
                    yield

                def rec_gen(c, h=h, qT=qT, wi=wi):
                    H = HS[c % NHS]
                    R = RS[c % NRS]
                    hb, rb = c % NHS, c % NRS
                    HK = lambda n: f"H{n}{hb}"
                    RK_ = lambda n: f"R{n}{rb}"
                    cs = slice(c * 128, (c + 1) * 128)
                    col = lambda tile: tile[:, c, h:h + 1]
                    Sc, Sn = Sst[c % 2], Sst[(c + 1) % 2]
                    Sck, Snk = f"Sst{c % 2}", f"Sst{(c + 1) % 2}"
                    Y1 = H["Y1"]
                    self.mm(ps[6][:, 0:128], H["wT"][:], Sc[:], rd=[HK("wT"), Sck], wr=["ps6"])
                    self.mm(ps[6][:, 128:256], qT[:, cs], Sc[:], rd=["qkvT0", Sck], wr=["ps6"])
                    self.tt(R["vnew"][:], Y1[:, 0:128], ps[6][:, 0:128], ALU.subtract, rd=[HK("Y1"), "ps6"], wr=[RK_("vnew")])
                    self.act(R["As"][:], ps[6][:, 128:256], AF.Copy, scale=col(eqs), rd=["ps6", "eqs"], wr=[RK_("As")])
                    yield
                    self.mm(ps[6][:, 384:512], H["kdec"][:], R["vnew"][:], rd=[HK("kdec"), RK_("vnew")], wr=["ps6"])
                    self.mm(ps[6][:, 256:384], H["attnT"][:], R["vnew"][:], rd=[HK("attnT"), RK_("vnew")], wr=["ps6"])
                    self.stt(Sn[:], Sc[:], col(etot), ps[6][:, 384:512], ALU.mult, ALU.add, rd=[Sck, "etot", "ps6"], wr=[Snk])
                    self.tt(R["ot"][:], R["As"][:], ps[6][:, 256:384], ALU.add, rd=[RK_("As"), "ps6"], wr=[RK_("ot")])
                    yield
                    for _ in self.out_gate_gen(R, RK_, zs_all[:, c, :], "zs_all", nw, "nw", oT[:, h, cs], ("oT", h, c)):
                        yield

                def rec_chain(cs_):
                    for c in cs_:
                        yield from rec_gen(c)

                def run_rr(gens):
                    gens = list(gens)
                    while gens:
                        for g in list(gens):
                            try:
                                next(g)
                            except StopIteration:
                                gens.remove(g)
                G = 4
                groups = [list(range(g0, g0 + G)) for g0 in range(0, NT, G)]
                run_rr([pre_gen(c) for c in groups[0]])
                for gi in range(len(groups)):
                    gens = []
                    if gi + 1 < len(groups):
                        gens += [pre_gen(c) for c in groups[gi + 1]]
                    gens.append(rec_chain(groups[gi]))
                    run_rr(gens)

    def out_gate_gen(self, d, K, zs_ap, zsk, nw, nwk, oT_dst, oTk, pbank=7, pcol=128):
        ps = self.ps
        pk = f"ps{pbank}"
        self.act(d["As"][:], d["ot"][:], AF.Square, rd=[K("ot"), K("As")], wr=[K("As"), K("ss")], accum=d["ss"][:])
        self.act(d["ss"][:], d["ss"][:], AF.Ln, bias=1e-6, scale=1.0 / 128.0, rd=[K("ss")], wr=[K("ss")])
        self.act(d["ss"][:], d["ss"][:], AF.Exp, scale=-0.5, rd=[K("ss")], wr=[K("ss")])
        yield
        self.stt(d["ot"][:], d["ot"][:], d["ss"][:, 0:1], nw[:], ALU.mult, ALU.mult, rd=[K("ot"), K("ss"), nwk], wr=[K("ot")])
        self.tt(d["ot"][:], d["ot"][:], zs_ap, ALU.mult, rd=[K("ot"), zsk], wr=[K("ot")], eng='pool')
        yield
        self.tr(ps[pbank][:, pcol:pcol + 128], d["ot"][:], rd=[K("ot")], wr=[pk])
        self.cp(oT_dst, ps[pbank][:, pcol:pcol + 128], rd=[pk], wr=[oTk], eng='act')
        yield

    def fox(self, l, graw, oT, XT):
        P = self.P
        I = self.I
        ps = self.ps
        BK = self.BK
        with ExitStack() as es:
            sb = lambda n, shp, dt=F32: self.sb(es, n, shp, dt)
            wv = sb("wv", [128, 8, 512], BF16)
            P.dma(wv[:], self.wcols(I['w_in'], l, OFF['fox_v'], OFF['fox_v'] + 512), writes=["wv"], q='pool')
            wqk = [sb(f"wqk{i}", [128, 8, 2, 128], BF16) for i in range(2)]

            def load_w(hp):
                i = hp % 2
                for j, nm in enumerate(['fox_q', 'fox_k']):
                    P.dma(wqk[i][:, :, j, :], self.wcols(I['w_in'], l, OFF[nm] + hp * 128, OFF[nm] + (hp + 1) * 128),
                          writes=[f"wqk{i}"], q='pool')
            load_w(0)
            fb = sb("fb", [128, NT, 8])
            P.dma(fb[:].rearrange("p a b -> p (a b)"), self.bcast(I['fb_r'][l, :]), writes=["fb"])
            vaug = sb("vaug", [128, NT, 8, 65], BF16)
            self.memset(vaug[:, :, :, 64:65], 1.0, wr=["vaug"])
            for t in range(NT):
                bank = ps[t % 2]
                pk = BK(t % 2)
                for k in range(8):
                    self.mm(bank[:, :], self.xT[:, k, t * 128:(t + 1) * 128], wv[:, k, :], start=(k == 0), stop=(k == 7),
                            rd=[XT[t], "wv"], wr=pk)
                self.cp(vaug[:, t, :, 0:64], bank[:, :].rearrange("p (a b) -> p a b", a=8), rd=pk, wr=["vaug"],
                        eng='act' if t % 2 else 'dve')
            logf = sb("logf", [128, NT, 8])
            lsum = sb("lsum", [128, NT, 8])
            cc = sb("cc", [128, NT, 8])
            cref = sb("cref", [128, NT, 8])
            self.tt(logf[:], graw[:, :, 8:16], fb[:], ALU.add, rd=["graw", "fb"], wr=["logf"])
            self.act(logf[:], logf[:], AF.Exp, scale=-1.0, rd=["logf"], wr=["logf"])
            self.act(logf[:], logf[:], AF.Ln, bias=1.0, rd=["logf"], wr=["logf"])
            self.ts(logf[:], logf[:], -1.0, None, ALU.mult, rd=["logf"], wr=["logf"])
            self.cp(lsum[:, 0, :], logf[:, 0, :], rd=["logf"], wr=["lsum"])
            for t in range(1, NT):
                self.tt(lsum[:, t, :], lsum[:, t - 1, :], logf[:, t, :], ALU.add, rd=["logf", "lsum"], wr=["lsum"])
            self.memset(cref[:, 0, :], 0.0, wr=["cref"])
            for t in range(NT):
                self.mm(ps[2][:, 0:8], self.tri[:], logf[:, t, :], start=True, stop=(t == 0), rd=["logf", "const"], wr=["ps2"])
                if t > 0:
                    self.mm(ps[2][:, 0:8], self.ones[:], lsum[:, t - 1, :], start=False, stop=True, rd=["lsum", "const"], wr=["ps2"])
                    self.mm(ps[2][:, 8:16], self.ones[:], lsum[:, t - 1, :], rd=["lsum", "const"], wr=["ps2"])
                    self.cp(cref[:, t, :], ps[2][:, 8:16], rd=["ps2"], wr=["cref"], eng='act')
                self.cp(cc[:, t, :], ps[2][:, 0:8], rd=["ps2"], wr=["cc"])
            biasm = sb("biasm", [128, 8, NT, NT])
            for h in range(8):
                for i in range(NT):
                    self.ts(biasm[:, h, i, 0:i + 1], cc[:, 0:i + 1, h], -1.0, cref[:, i, h:h + 1], ALU.mult, ALU.add,
                            rd=["cc", "cref"], wr=["biasm"])
            qkT = [sb(f"qkT{i}", [128, 2, S], BF16) for i in range(2)]
            ptile = [sb(f"ptile{i}", [128, 512], BF16) for i in range(3)]
            opair = sb("opair", [128, NT, 128])
            rinv = [sb(f"rinv{i}", [128, 1]) for i in range(2)]
            pcount = 0
            ocount = 0
            for hp in range(4):
                wi = hp % 2
                if hp + 1 < 4:
                    load_w(hp + 1)
                for j in range(2):
                    for tg in range(4):
                        bank = ps[tg % 2]
                        pk = BK(tg % 2)
                        for k in range(8):
                            self.mm(bank[:, :], wqk[wi][:, k, j, :], self.xT[:, k, tg * 512:(tg + 1) * 512],
                                    start=(k == 0), stop=(k == 7), rd=[f"wqk{wi}"] + XT[tg * 4:(tg + 1) * 4], wr=pk)
                        self.cp(qkT[wi][:, j, tg * 512:(tg + 1) * 512], bank[:, :], rd=pk, wr=[f"qkT{wi}"],
                                eng='act' if tg % 2 else 'dve')
                for hl in range(2):
                    h = hp * 2 + hl
                    prt = slice(hl * 64, (hl + 1) * 64)
                    for g in range(4):
                        ob = ps[4 + (g % 2)]
                        obk = BK(4 + (g % 2))
                        self.memset(ob[:, 0:260], 0.0, wr=obk)
                        def emit_qk(j, g=g, h=h, prt=prt, wi=wi):
                            nonlocal pcount
                            i_lo = max(j, 4 * g)
                            ncol = (4 * g + 4 - i_lo) * 128
                            sbank = ps[2 + (j % 2)]
                            sk = BK(2 + (j % 2))
                            self.mm(sbank[:, 0:ncol], qkT[wi][prt, 1, j * 128:(j + 1) * 128],
                                    qkT[wi][prt, 0, i_lo * 128:(4 * g + 4) * 128], rd=[f"qkT{wi}"], wr=sk)
                            pt = ptile[pcount % 3]
                            ptk = f"ptile{pcount % 3}"
                            pcount += 1
                            for i in range(i_lo, 4 * g + 4):
                                o = (i - i_lo) * 128
                                self.act(pt[:, o:o + 128], sbank[:, o:o + 128], AF.Exp, bias=biasm[:, h, i, j:j + 1], scale=0.125,
                                         rd=list(sk) + ["biasm"], wr=[(ptk, o // 128)])
                            if j >= 4 * g:
                                self.tt(pt[:, 0:128], pt[:, 0:128], self.causalb[:], ALU.mult, rd=[(ptk, 0), "const"], wr=[(ptk, 0)], eng='pool')
                            return pt, ptk, i_lo

                        def emit_pv(j, pt, ptk, i_lo, g=g, h=h, ob=ob, obk=obk):
                            for i in range(i_lo, 4 * g + 4):
                                o = (i - i_lo) * 128
                                oc = (i - 4 * g) * 65
                                self.mm(ob[:, oc:oc + 65], pt[:, o:o + 128], vaug[:, j, h, :], start=False, stop=False,
                                        rd=[(ptk, o // 128), "vaug"], wr=obk, skip=True, inc=(i == 4 * g + 3))
                        nj = 4 * g + 4
                        cur = emit_qk(0)
                        for j in range(nj):
                            nxt_ = emit_qk(j + 1) if j + 1 < nj else None
                            emit_pv(j, *cur)
                            cur = nxt_
                        for ii in range(4):
                            i = 4 * g + ii
                            oc = ii * 65
                            ri = rinv[ocount % 2]
                            rik = f"rinv{ocount % 2}"
                            ocount += 1
                            P.op('dve', lambda e, ri=ri, ob=ob, oc=oc: e.reciprocal(out=ri[:], in_=ob[:, oc + 64:oc + 65]), obk, [rik])
                            self.ts(opair[:, i, hl * 64:(hl + 1) * 64], ob[:, oc:oc + 64], ri[:, 0:1], None, ALU.mult,
                                    rd=list(obk) + [rik], wr=[("opair", i)])
                for t in range(NT):
                    r = t % 4
                    self.tr(ps[7][:, r * 128:(r + 1) * 128], opair[:, t, :], rd=[("opair", t)], wr=[self.RK(7, r)])
                    self.cp(oT[:, 4 + hp, t * 128:(t + 1) * 128], ps[7][:, r * 128:(r + 1) * 128], rd=[self.RK(7, r)],
                            wr=[("oT", 4 + hp, t)], eng='act')

    def gla(self, l, graw, oT, XT):
        P = self.P
        I = self.I
        ps = self.ps
        BK = self.BK
        with ExitStack() as es:
            sb = lambda n, shp, dt=F32: self.sb(es, n, shp, dt)
            wq = sb("wq", [128, 8, 256], BF16)
            wk = sb("wk", [128, 8, 256], BF16)
            wv = sb("wv", [128, 8, 512], BF16)
            wr_ = sb("wr", [128, 8, 512], BF16)
            for tl, nm, n in [(wq, 'gla_q', 256), (wk, 'gla_k', 256), (wv, 'gla_v', 512), (wr_, 'gla_r', 512)]:
                P.dma(tl[:], self.wcols(I['w_in'], l, OFF[nm], OFF[nm] + n), writes=["glaw"], q='pool')
            w2 = sb("w2", [16, 256])
            P.dma(w2[:], I['gla_w_gate2'][l], writes=["w2"])
            bg = sb("bg", [128, 256])
            P.dma(bg[:], self.bcast(I['gla_b_gate'][l, :]), writes=["bg"])
            nw = sb("nw", [128, 128])
            P.dma(nw[:], self.bcast(I['gla_norm_w'][l, :]), writes=["nwc"])
            lrT = sb("lrT", [16, 128])
            NB = 3
            bufs = []
            for b_ in range(NB):
                d = {}
                for n in ["la", "cum", "ecum", "encum", "edk", "qt", "kt", "kd"]:
                    d[n] = sb(f"{n}{b_}", [128, 256])
                d["v"] = sb(f"v{b_}", [128, 512])
                d["zs4"] = sb(f"zs4{b_}", [128, 512])
                d["qtT"] = sb(f"qtT{b_}", [128, 2, 128])
                d["ktT"] = sb(f"ktT{b_}", [128, 2, 128])
                d["cd"] = sb(f"cd{b_}", [128, 2])
                bufs.append(d)
            RB = []
            for b_ in range(4):
                d = {n: sb(f"gR{n}{b_}", [128, 128]) for n in ["attnT", "ot", "As"]}
                d["ss"] = sb(f"gRss{b_}", [128, 1])
                RB.append(d)
            Sg = [[sb(f"Sg{p}_{i}", [128, 128]) for i in range(2)] for p in range(2)]
            for p in range(2):
                self.memset(Sg[p][0][:], 0.0, wr=[f"Sg{p}_0_0", f"Sg{p}_1_0"])

            def pre_gen(c):
                d = bufs[c % NB]
                b_ = c % NB
                K = lambda n: f"g{n}{b_}"
                cs = slice(c * 128, (c + 1) * 128)
                self.tr(ps[0][0:16, 0:128], graw[:, c, 16:32], rd=["graw"], wr=["ps0"])
                self.cp(lrT[:], ps[0][0:16, 0:128], rd=["ps0"], wr=["lrT"])
                self.mm(ps[0][:, 256:512], lrT[:], w2[:], rd=["lrT", "w2"], wr=["ps0"])
                self.tt(d["la"][:], ps[0][:, 256:512], bg[:], ALU.add, rd=["ps0", "bg"], wr=[K("la")])
                yield
                self.act(d["la"][:], d["la"][:], AF.Exp, scale=-1.0, rd=[K("la")], wr=[K("la")])
                self.act(d["la"][:], d["la"][:], AF.Ln, bias=1.0, rd=[K("la")], wr=[K("la")])
                self.ts(d["la"][:], d["la"][:], -1.0 / 16.0, None, ALU.mult, rd=[K("la")], wr=[K("la")])
                yield
                self.mm(ps[1][:, 0:256], self.tri[:], d["la"][:], rd=["const", K("la")], wr=["ps1"])
                self.mm(ps[1][:, 256:512], self.ones[:], d["la"][:], rd=["const", K("la")], wr=["ps1"])
                for p in range(2):
                    self.mm(ps[2][:, p:p + 1], d["la"][:, p * 128:(p + 1) * 128], self.ones[:, 0:1], rd=[K("la"), "const"], wr=["ps2"])
                self.cp(d["cum"][:], ps[1][:, 0:256], rd=["ps1"], wr=[K("cum")])
                self.tt(d["edk"][:], ps[1][:, 256:512], d["cum"][:], ALU.subtract, rd=["ps1", K("cum")], wr=[K("edk")])
                self.act(d["cd"][:], ps[2][:, 0:2], AF.Exp, rd=["ps2"], wr=[K("cd")])
                yield
                self.act(d["ecum"][:], d["cum"][:], AF.Exp, rd=[K("cum")], wr=[K("ecum")])
                self.act(d["encum"][:], d["cum"][:], AF.Exp, scale=-1.0, rd=[K("cum")], wr=[K("encum")])
                self.act(d["edk"][:], d["edk"][:], AF.Exp, rd=[K("edk")], wr=[K("edk")])
                yield
                for k in range(8):
                    self.mm(ps[3][:, 0:256], self.xT[:, k, cs], wq[:, k, :], start=(k == 0), stop=(k == 7), rd=[XT[c], "glaw"], wr=["ps3"])
                for k in range(8):
                    self.mm(ps[3][:, 256:512], self.xT[:, k, cs], wk[:, k, :], start=(k == 0), stop=(k == 7), rd=[XT[c], "glaw"], wr=["ps3"])
                self.stt(d["qt"][:], ps[3][:, 0:256], 0.125, d["ecum"][:], ALU.mult, ALU.mult, rd=["ps3", K("ecum")], wr=[K("qt")])
                self.tt(d["kt"][:], ps[3][:, 256:512], d["encum"][:], ALU.mult, rd=["ps3", K("encum")], wr=[K("kt")])
                self.tt(d["kd"][:], ps[3][:, 256:512], d["edk"][:], ALU.mult, rd=["ps3", K("edk")], wr=[K("kd")])
                yield
                for k in range(8):
                    self.mm(ps[4][:, :], self.xT[:, k, cs], wv[:, k, :], start=(k == 0), stop=(k == 7), rd=[XT[c], "glaw"], wr=["ps4"])
                self.cp(d["v"][:], ps[4][:, :], rd=["ps4"], wr=[K("v")], eng='act')
                yield
                for k in range(8):
                    self.mm(ps[4][:, :], self.xT[:, k, cs], wr_[:, k, :], start=(k == 0), stop=(k == 7), rd=[XT[c], "glaw"], wr=["ps4"])
                self.act(d["zs4"][:], ps[4][:, :], AF.Silu, rd=["ps4"], wr=[K("zs4")])
                yield
                for p in range(2):
                    self.tr(ps[5][:, p * 128:(p + 1) * 128], d["qt"][:, p * 128:(p + 1) * 128], rd=[K("qt")], wr=["ps5"])
                    self.tr(ps[5][:, (2 + p) * 128:(3 + p) * 128], d["kt"][:, p * 128:(p + 1) * 128], rd=[K("kt")], wr=["ps5"])
                self.cp(d["qtT"][:], ps[5][:, 0:256].rearrange("p (a b) -> p a b", a=2), rd=["ps5"], wr=[K("qtT")], eng='act')
                self.cp(d["ktT"][:], ps[5][:, 256:512].rearrange("p (a b) -> p a b", a=2), rd=["ps5"], wr=[K("ktT")])
                yield

            def rec_head_gen(c, h):
                d = bufs[c % NB]
                b_ = c % NB
                K = lambda n: f"g{n}{b_}"
                cs = slice(c * 128, (c + 1) * 128)
                p, hl = h // 2, h % 2
                prt = slice(hl * 64, (hl + 1) * 64)
                R = RB[h]
                RK_ = lambda n: f"gR{n}{h}"
                Sc, Sn = Sg[p][c % 2], Sg[p][(c + 1) % 2]
                Sck, Snk = f"Sg{p}_{hl}_{c % 2}", f"Sg{p}_{hl}_{(c + 1) % 2}"
                pb = ps[6 + h % 2]
                pbk = f"ps{6 + h % 2}"
                self.mm(pb[:, 0:128], d["ktT"][prt, p, :], d["qtT"][prt, p, :], rd=[K("ktT"), K("qtT")], wr=[pbk])
                self.mm(pb[prt, 256:384], d["kd"][:, h * 64:(h + 1) * 64], d["v"][:, h * 128:(h + 1) * 128],
                        rd=[K("kd"), K("v")], wr=[pbk])
                self.tt(R["attnT"][:], pb[:, 0:128], self.causal[:], ALU.mult, rd=[pbk, "const"], wr=[RK_("attnT")])
                self.stt(Sn[prt, :], Sc[prt, :], d["cd"][prt, p:p + 1], pb[prt, 256:384], ALU.mult, ALU.add,
                         rd=[Sck, K("cd"), pbk], wr=[Snk])
                yield
                self.mm(pb[:, 128:256], d["qtT"][prt, p, :], Sc[prt, :], start=True, stop=False, rd=[K("qtT"), Sck], wr=[pbk])
                self.mm(pb[:, 128:256], R["attnT"][:], d["v"][:, h * 128:(h + 1) * 128], start=False, stop=True,
                        rd=[RK_("attnT"), K("v")], wr=[pbk])
                self.cp(R["ot"][:], pb[:, 128:256], rd=[pbk], wr=[RK_("ot")], eng='act')
                yield
                for _ in self.out_gate_gen(R, RK_, d["zs4"][:, h * 128:(h + 1) * 128], K("zs4"), nw, "nwc",
                                           oT[:, 8 + h, cs], ("oT", 8 + h, c), pbank=6 + h % 2, pcol=384):
                    yield

            def run_rr(gens):
                gens = list(gens)
                while gens:
                    for g in list(gens):
                        try:
                            next(g)
                        except StopIteration:
                            gens.remove(g)
            run_rr([pre_gen(0), pre_gen(1)])
            for c in range(NT):
                gens = [rec_head_gen(c, h) for h in range(4)]
                if c + 2 < NT:
                    gens.append(pre_gen(c + 2))
                run_rr(gens)

    def merge(self, l, s, src, dst, oT, XT):
        P = self.P
        I = self.I
        ps = self.ps
        BK = self.BK
        row0 = s * S
        with ExitStack() as es:
            sb = lambda n, shp, dt=F32: self.sb(es, n, shp, dt)
            self.alloc_io(es)
            wm = [sb(f"wm{i}", [128, 8, 512], BF16) for i in range(2)]
            P.dma(wm[0][:], self.wcols(I['w_in'], l, OFF['merge'], OFF['merge'] + 512), writes=["wm0"], q='pool')
            pw = [sb(f"pw{j}", [128, 4, D], BF16) for j in range(3)]
            for j, nm in enumerate(['p_gdn', 'p_fox', 'p_gla']):
                P.dma(pw[j][:], I[nm][l].rearrange("(c p) n -> p c n", p=128), writes=["pw"], q='pool')
            wo = sb("wo", [128, 8, D], BF16)
            P.dma(wo[:], I['w_out'][l].rearrange("(k p) n -> p k n", p=128), writes=["wo"], q='pool')
            bm = sb("bm", [128, 3 * D])
            P.dma(bm[:], self.bcast(I['b_merge'][l, :]), writes=["bm"])
            self.load_ln('ln1_g', 'ln1_b', l)
            merged = sb("merged", [128, 4, D])
            gt = [sb(f"gt{i}", [128, 512]) for i in range(2)]
            mT4 = [sb(f"mT4_{i}", [128, 8, 128], BF16) for i in range(4)]
            pendingB = []
            blocks = [(j, half) for half in range(2) for j in range(3)]
            nload = 0

            def load_wm(j, half):
                nonlocal nload
                i = nload % 2
                nload += 1
                c0 = OFF['merge'] + j * D + half * 512
                P.dma(wm[i][:], self.wcols(I['w_in'], l, c0, c0 + 512), writes=[f"wm{i}"], q='pool')
            seq = [(tg, j, half) for tg in range(4) for (j, half) in blocks]
            nload = 1
            cnt = 0
            for idx, (tg, j, half) in enumerate(seq):
                wi = idx % 2
                if idx + 1 < len(seq):
                    load_wm(seq[idx + 1][1], seq[idx + 1][2])
                for tt_ in range(4):
                    t = tg * 4 + tt_
                    cs = slice(t * 128, (t + 1) * 128)
                    gb = ps[cnt % 2]
                    gbk = BK(cnt % 2)
                    bb = ps[2 + cnt % 2]
                    bbk = BK(2 + cnt % 2)
                    g_ = gt[cnt % 2]
                    gk = f"gt{cnt % 2}"
                    cnt += 1
                    for k in range(8):
                        self.mm(gb[:, :], self.xT[:, k, cs], wm[wi][:, k, :], start=(k == 0), stop=(k == 7), rd=[XT[t], f"wm{wi}"], wr=gbk)
                    for c4 in range(4):
                        self.mm(bb[:, :], oT[:, 4 * j + c4, cs], pw[j][:, c4, half * 512:(half + 1) * 512], start=(c4 == 0), stop=(c4 == 3),
                                rd=[("oT", 4 * j + c4, t), "pw"], wr=bbk)
                    self.tt(g_[:], gb[:, :], bm[:, j * D + half * 512: j * D + (half + 1) * 512], ALU.add, rd=list(gbk) + ["bm"], wr=[gk])
                    self.act(g_[:], g_[:], AF.Sigmoid, rd=[gk], wr=[gk])
                    mslice = merged[:, tt_, half * 512:(half + 1) * 512]
                    if j == 0:
                        self.tt(mslice, g_[:], bb[:, :], ALU.mult, rd=[gk] + list(bbk), wr=[("merged", tt_)])
                    else:
                        self.tt(g_[:], g_[:], bb[:, :], ALU.mult, rd=[gk] + list(bbk), wr=[gk])
                        self.tt(mslice, mslice, g_[:], ALU.add, rd=[gk, ("merged", tt_)], wr=[("merged", tt_)], eng='pool')
                if pendingB:
                    tt2, t2 = pendingB.pop(0)
                    self.finish_B(mT4[tt2], f"mT4_{tt2}", wo, "wo", src, dst, row0 + t2 * 128, t2)
                if (j, half) == blocks[-1]:
                    for tt_ in range(4):
                        self.finish_A(merged[:, tt_, :], ("merged", tt_), mT4[tt_], f"mT4_{tt_}")
                        pendingB.append((tt_, tg * 4 + tt_))
            while pendingB:
                tt2, t2 = pendingB.pop(0)
                self.finish_B(mT4[tt2], f"mT4_{tt2}", wo, "wo", src, dst, row0 + t2 * 128, t2)

    def finish_A(self, m_ap, mkey, mT, mTk):
        ps = self.ps
        BK = self.BK
        for half in range(2):
            for kk in range(4):
                k = half * 4 + kk
                self.tr(ps[4 + half][:, kk * 128:(kk + 1) * 128], m_ap[:, k * 128:(k + 1) * 128], rd=[mkey], wr=[self.RK(4 + half, kk)])
            self.cp(mT[:, half * 4:(half + 1) * 4, :], ps[4 + half][:, :].rearrange("p (a b) -> p a b", a=4), rd=BK(4 + half), wr=[mTk],
                    eng='act' if half else 'dve')

    def finish_B(self, mT, mTk, wo, wok, src, dst, row, t):
        P = self.P
        ps = self.ps
        BK = self.BK
        yt = self.ytile[t % 2]
        yk = f"ytile{t % 2}"
        xr = self.xin[t % 2]
        xk = f"xin{t % 2}"
        P.dma(xr[:], src[row:row + 128, :], writes=[xk])
        for half in range(2):
            for k in range(8):
                self.mm(ps[6 + half][:, :], mT[:, k, :], wo[:, k, half * 512:(half + 1) * 512], start=(k == 0), stop=(k == 7),
                        rd=[mTk, wok], wr=BK(6 + half))
            self.cp(yt[:, half * 512:(half + 1) * 512], ps[6 + half][:, :], rd=BK(6 + half), wr=[yk], eng='act')
        self.resid_ln_store(yt, yk, xr, xk, dst[row:row + 128, :])

    def finish_tile(self, m_ap, mkey, mT, wo, wok, src, dst, row, t):
        self.finish_A(m_ap, mkey, mT, "mT")
        self.finish_B(mT, "mT", wo, wok, src, dst, row, t)

    def xattn(self, l, s, src, dst):
        P = self.P
        I = self.I
        ps = self.ps
        BK = self.BK
        row0 = s * S
        XT = [("xT", t) for t in range(NT)]
        with ExitStack() as es:
            sb = lambda n, shp, dt=F32: self.sb(es, n, shp, dt)
            self.alloc_io(es, nxin=4)
            wq = sb("xwq", [128, 8, D], BF16)
            wo = sb("xwo", [128, 8, D], BF16)
            wkv = [sb(f"wkv{i}", [128, 8, 512], BF16) for i in range(4)]
            for blk in range(4):
                P.dma(wkv[blk][:], self.wcols(I['xa_wkv'], l, blk * 512, (blk + 1) * 512), writes=[f"wkv{blk}"], q='pool')
            P.dma(wq[:], I['xa_wq'][l].rearrange("(k p) n -> p k n", p=128), writes=["xwq"], q='pool')
            P.dma(wo[:], I['xa_wo'][l].rearrange("(k p) n -> p k n", p=128), writes=["xwo"], q='pool')
            self.load_ln('ln2_g', 'ln2_b', l)
            memT = sb("memT", [128, 8, 256], BF16)
            kT = sb("kT", [128, 8, 256], BF16)
            vaug = sb("xvaug", [128, 2, 4, 257], BF16)
            self.memset(vaug[:, :, :, 256:257], 1.0, wr=["xvaug"])
            for mt in range(2):
                buf = self.xin[mt % 2]
                bk = f"xin{mt % 2}"
                P.dma(buf[:], I['mem'][s * 256 + mt * 128: s * 256 + (mt + 1) * 128, :], writes=[bk])
                for half in range(2):
                    for kk in range(4):
                        k = half * 4 + kk
                        self.tr(ps[half][:, kk * 128:(kk + 1) * 128], buf[:, k * 128:(k + 1) * 128], rd=[bk], wr=[self.RK(half, kk)])
                    self.cp(memT[:, half * 4:(half + 1) * 4, mt * 128:(mt + 1) * 128], ps[half][:, :].rearrange("p (a b) -> p a b", a=4),
                            rd=BK(half), wr=["memT"], eng='act' if half else 'dve')
            for blk in range(4):
                wi = blk
                if blk < 2:
                    for cc_ in range(4):
                        c = blk * 4 + cc_
                        bank = ps[2 + cc_ % 2]
                        for k in range(8):
                            self.mm(bank[:, 0:256], wkv[wi][:, k, cc_ * 128:(cc_ + 1) * 128], memT[:, k, :], start=(k == 0), stop=(k == 7),
                                    rd=[f"wkv{wi}", "memT"], wr=BK(2 + cc_ % 2))
                        self.cp(kT[:, c, :], bank[:, 0:256], rd=BK(2 + cc_ % 2), wr=["kT"], eng='act' if cc_ % 2 else 'dve')
                else:
                    vb = blk - 2
                    for mc in range(2):
                        bank = ps[2 + mc]
                        for k in range(8):
                            self.mm(bank[:, :], memT[:, k, mc * 128:(mc + 1) * 128], wkv[wi][:, k, :], start=(k == 0), stop=(k == 7),
                                    rd=[f"wkv{wi}", "memT"], wr=BK(2 + mc))
                        self.cp(vaug[:, mc, 2 * vb:2 * vb + 2, 0:256], bank[:, :].rearrange("p (a b) -> p a b", a=2), rd=BK(2 + mc),
                                wr=["xvaug"], eng='act' if mc else 'dve')
            self.build_xT(src, row0)
            qT = [sb(f"xqT{i}", [128, 8, 512], BF16) for i in range(2)]
            pt = [sb(f"xpt{i}", [128, 2, 512], BF16) for i in range(2)]
            xo = sb("xo", [128, 4, D])
            rinv = [sb(f"xrinv{i}", [128, 1]) for i in range(2)]
            oTt = sb("oTt", [128, 8, 128], BF16)
            pc = 0
            rc = 0
            def qproj_chunk(tg, c):
                q_ = qT[tg % 2]
                qk = f"xqT{tg % 2}"
                bank = ps[c % 2]
                for k in range(8):
                    self.mm(bank[:, :], wq[:, k, c * 128:(c + 1) * 128], self.xT[:, k, tg * 512:(tg + 1) * 512], start=(k == 0), stop=(k == 7),
                            rd=["xwq"] + XT[tg * 4:(tg + 1) * 4], wr=BK(c % 2))
                self.cp(q_[:, c, :], bank[:, :], rd=BK(c % 2), wr=[(qk, c)], eng='act' if c % 2 else 'dve')
            for c in range(8):
                qproj_chunk(0, c)
            for tg in range(4):
                q_ = qT[tg % 2]
                qk = f"xqT{tg % 2}"
                for h in range(4):
                    p_ = pt[pc % 2]
                    pk_ = f"xpt{pc % 2}"
                    pc += 1
                    for mc in range(2):
                        bank = ps[2 + mc]
                        for dc in range(2):
                            self.mm(bank[:, :], kT[:, 2 * h + dc, mc * 128:(mc + 1) * 128], q_[:, 2 * h + dc, :], start=(dc == 0), stop=(dc == 1),
                                    rd=["kT", (qk, 2 * h + dc)], wr=BK(2 + mc))
                        self.act(p_[:, mc, :], bank[:, :], AF.Exp, scale=1.0 / 16.0, rd=BK(2 + mc), wr=[pk_])
                    if tg + 1 < 4:
                        qproj_chunk(tg + 1, 2 * h)
                        qproj_chunk(tg + 1, 2 * h + 1)
                    for tt_ in range(4):
                        ob = ps[4 + tt_ % 2]
                        obk = BK(4 + tt_ % 2)
                        for mc in range(2):
                            self.mm(ob[:, 0:257], p_[:, mc, tt_ * 128:(tt_ + 1) * 128], vaug[:, mc, h, :], start=(mc == 0), stop=(mc == 1),
                                    rd=[pk_, "xvaug"], wr=obk)
                        ri = rinv[rc % 2]
                        rik = f"xrinv{rc % 2}"
                        rc += 1
                        P.op('dve', lambda e, ri=ri, ob=ob: e.reciprocal(out=ri[:], in_=ob[:, 256:257]), obk, [rik])
                        self.ts(xo[:, tt_, h * 256:(h + 1) * 256], ob[:, 0:256], ri[:, 0:1], None, ALU.mult, rd=list(obk) + [rik],
                                wr=[("xo", tt_)])
                for tt_ in range(4):
                    t = tg * 4 + tt_
                    self.finish_tile(xo[:, tt_, :], ("xo", tt_), oTt, wo, "xwo", src, dst, row0 + t * 128, t)

    def moe(self, l, s, src, dst):
        P = self.P
        I = self.I
        ps = self.ps
        BK = self.BK
        row0 = s * S
        XT = [("xT", t) for t in range(NT)]
        with ExitStack() as es:
            sb = lambda n, shp, dt=F32: self.sb(es, n, shp, dt)
            self.alloc_io(es, ytile=False, nxin=4)
            wr_ = sb("mwr", [128, 8, 36])
            P.dma(wr_[:], I['moe_wr_r'][l].rearrange("p (k n) -> p k n", k=8), writes=["gate_w"])
            br = sb("mbr", [128, 36])
            P.dma(br[:], self.bcast(I['moe_br'][l, :]), writes=["mbr"])
            self.load_ln('ln3_g', 'ln3_b', l)
            rl = sb("rl", [128, NT, 36])
            yacc = sb("yacc", [128, NT, D])
            with ExitStack() as e0:
                xTf = self.sb(e0, "xTf", [128, 8, 128], F32)
                self.build_xT(src, row0, wr_, rl, 36, xTf)
                P.barrier()
            G = sb("G", [128, NT, 4])
            gmax = sb("gmax", [128, NT, 1])
            gsum = sb("gsum", [128, NT, 1])
            goh = sb("goh", [128, NT, 4])
            EL = sb("EL", [128, NT, 32])
            m1 = sb("m1", [128, NT, 1])
            m2 = sb("m2", [128, NT, 1])
            oh1 = sb("oh1", [128, NT, 32])
            oh2 = sb("oh2", [128, NT, 32])
            EL2 = sb("EL2", [128, NT, 32])
            wa = sb("wa", [128, NT, 1])
            wb = sb("wb", [128, NT, 1])
            Wc = sb("Wc", [128, NT, 32])
            for t in range(NT):
                self.tt(rl[:, t, :], rl[:, t, :], br[:], ALU.add, rd=["graw", "mbr"], wr=["graw"])
            self.cp(G[:], rl[:, :, 0:4], rd=["graw"], wr=["G"])
            self.cp(EL[:], rl[:, :, 4:36], rd=["graw"], wr=["EL"], eng='act')
            P.op('dve', lambda e: e.tensor_reduce(out=gmax[:], in_=G[:], axis=mybir.AxisListType.X, op=ALU.max), ["G"], ["gmax"])
            for t in range(NT):
                self.ts(goh[:, t, :], G[:, t, :], gmax[:, t, :], None, ALU.is_equal, rd=["G", "gmax"], wr=["goh"])
                self.ts(G[:, t, :], G[:, t, :], gmax[:, t, :], None, ALU.subtract, rd=["G", "gmax"], wr=["G"])
            self.act(G[:], G[:], AF.Exp, rd=["G"], wr=["G"])
            P.op('dve', lambda e: e.tensor_reduce(out=gsum[:], in_=G[:], axis=mybir.AxisListType.X, op=ALU.add), ["G"], ["gsum"])
            P.op('dve', lambda e: e.reciprocal(out=gsum[:], in_=gsum[:]), ["gsum"], ["gsum"])
            self.ts(goh[:], goh[:], -1.0, 30000.0, ALU.add, ALU.mult, rd=["goh"], wr=["goh"])
            for t in range(NT):
                for g in range(4):
                    self.ts(EL[:, t, g * 8:(g + 1) * 8], EL[:, t, g * 8:(g + 1) * 8], goh[:, t, g:g + 1], None, ALU.add,
                            rd=["EL", "goh"], wr=["EL"], eng='pool' if g % 2 else 'dve')
            P.op('dve', lambda e: e.tensor_reduce(out=m1[:], in_=EL[:], axis=mybir.AxisListType.X, op=ALU.max), ["EL"], ["m1"])
            for t in range(NT):
                self.ts(oh1[:, t, :], EL[:, t, :], m1[:, t, :], None, ALU.is_equal, rd=["EL", "m1"], wr=["oh1"])
            self.stt(EL2[:], oh1[:], -30000.0, EL[:], ALU.mult, ALU.add, rd=["oh1", "EL"], wr=["EL2"])
            P.op('dve', lambda e: e.tensor_reduce(out=m2[:], in_=EL2[:], axis=mybir.AxisListType.X, op=ALU.max), ["EL2"], ["m2"])
            for t in range(NT):
                self.ts(oh2[:, t, :], EL2[:, t, :], m2[:, t, :], None, ALU.is_equal, rd=["EL2", "m2"], wr=["oh2"])
            self.tt(m2[:], m2[:], m1[:], ALU.subtract, rd=["m1", "m2"], wr=["m2"])
            self.act(m2[:], m2[:], AF.Exp, rd=["m2"], wr=["m2"])
            self.ts(m1[:], m2[:], 1.0, None, ALU.add, rd=["m2"], wr=["m1"])
            P.op('dve', lambda e: e.reciprocal(out=m1[:], in_=m1[:]), ["m1"], ["m1"])
            self.tt(wa[:], m1[:], gsum[:], ALU.mult, rd=["m1", "gsum"], wr=["wa"])
            self.tt(wb[:], wa[:], m2[:], ALU.mult, rd=["wa", "m2"], wr=["wb"])
            for t in range(NT):
                self.ts(Wc[:, t, :], oh1[:, t, :], wa[:, t, :], None, ALU.mult, rd=["oh1", "wa"], wr=["Wc"])
                self.stt(Wc[:, t, :], oh2[:, t, :], wb[:, t, :], Wc[:, t, :], ALU.mult, ALU.add, rd=["oh2", "wb", "Wc"], wr=["Wc"])
            wg = [sb(f"wg{i}", [128, 8, 256], BF16) for i in range(2)]
            wu = [sb(f"wu{i}", [128, 8, 256], BF16) for i in range(2)]
            wd = [sb(f"wd{i}", [128, 2, D], BF16) for i in range(2)]
            sg = [sb(f"sg{i}", [128, 512]) for i in range(2)]
            hT = [sb(f"hT{i}", [128, 2, 512], BF16) for i in range(2)]

            def load_e(e):
                i = e % 2
                P.dma(wg[i][:], I['moe_w_gate'][l, e].rearrange("(k p) n -> p k n", p=128), writes=[f"wg{i}"], q='pool')
                P.dma(wu[i][:], I['moe_w_up'][l, e].rearrange("(k p) n -> p k n", p=128), writes=[f"wu{i}"], q='pool')
                P.dma(wd[i][:], I['moe_w_down'][l, e].rearrange("(c p) n -> p c n", p=128), writes=[f"wd{i}"], q='pool')
            load_e(0)
            hc = 0
            pending = None

            def ln_tiles(tg):
                for t in range(tg * 4, tg * 4 + 4):
                    xr = self.xin[t % 4]
                    xk = f"xin{t % 4}"
                    P.dma(xr[:], src[row0 + t * 128: row0 + (t + 1) * 128, :], writes=[xk])
                    self.resid_ln_store_keyed(yacc[:, t, :], [("yacc", t, 0), ("yacc", t, 1)], xr, xk,
                                              dst[row0 + t * 128: row0 + (t + 1) * 128, :], t)

            ytmp = [sb(f"ytmp{i}", [128, 512]) for i in range(2)]
            ycnt = [0]

            def make_y(e, tg, h_, hk, wi):
                def emit_y(tiles):
                    for tt_ in tiles:
                        t = tg * 4 + tt_
                        for half in range(2):
                            bi = 4 + (2 * tt_ + half) % 4
                            bank = ps[bi]
                            bkk = BK(bi)
                            for f in range(2):
                                self.mm(bank[:, :], h_[:, f, tt_ * 128:(tt_ + 1) * 128], wd[wi][:, f, half * 512:(half + 1) * 512],
                                        start=(f == 0), stop=(f == 1), rd=[hk, f"wd{wi}"], wr=bkk)
                            ysl = yacc[:, t, half * 512:(half + 1) * 512]
                            wcol = Wc[:, t, e:e + 1]
                            yk = ("yacc", t, half)
                            if e == 0:
                                self.ts(ysl, bank[:, :], wcol, None, ALU.mult, rd=list(bkk) + ["Wc"], wr=[yk])
                            elif half == 0:
                                self.stt(ysl, bank[:, :], wcol, ysl, ALU.mult, ALU.add, rd=list(bkk) + ["Wc", yk], wr=[yk])
                            else:
                                yt_ = ytmp[ycnt[0] % 2]
                                ytk = f"ytmp{ycnt[0] % 2}"
                                ycnt[0] += 1
                                self.act(yt_[:], bank[:, :], AF.Copy, scale=wcol, rd=list(bkk) + ["Wc"], wr=[ytk])
                                self.tt(ysl, ysl, yt_[:], ALU.add, rd=[ytk, yk], wr=[yk], eng='pool')
                return emit_y
            for e in range(32):
                wi = e % 2
                for tg in range(4):
                    ts_ = slice(tg * 512, (tg + 1) * 512)
                    h_ = hT[hc % 2]
                    hk = f"hT{hc % 2}"
                    hc += 1
                    for f in range(2):
                        for k in range(8):
                            self.mm(ps[f][:, :], wg[wi][:, k, f * 128:(f + 1) * 128], self.xT[:, k, ts_], start=(k == 0), stop=(k == 7),
                                    rd=[f"wg{wi}"] + XT[tg * 4:(tg + 1) * 4], wr=BK(f))
                        for k in range(8):
                            self.mm(ps[2 + f][:, :], wu[wi][:, k, f * 128:(f + 1) * 128], self.xT[:, k, ts_], start=(k == 0), stop=(k == 7),
                                    rd=[f"wu{wi}"] + XT[tg * 4:(tg + 1) * 4], wr=BK(2 + f))
                        s_ = sg[f]
                        sk = f"sg{f}"
                        self.act(s_[:], ps[f][:, :], AF.Silu, rd=BK(f), wr=[sk])
                        self.tt(h_[:, f, :], s_[:], ps[2 + f][:, :], ALU.mult, rd=[sk] + list(BK(2 + f)), wr=[hk])
                        if pending is not None:
                            pending([0, 1] if f == 0 else [2, 3])
                    if e == 31 and tg > 0:
                        ln_tiles(tg - 1)
                    if tg == 0 and e + 1 < 32 and not _os.environ.get('MOE_NOLOAD'):
                        load_e(e + 1)
                    pending = make_y(e, tg, h_, hk, wi)
            if pending is not None:
                pending([0, 1, 2, 3])
            ln_tiles(3)

    def resid_ln_store_keyed(self, yt_ap, ykeys, xres, xkey, dst_rows, t):
        P = self.P
        yk = ykeys
        self.stt(yt_ap, xres[:], ALPHA, yt_ap, ALU.mult, ALU.add, rd=list(yk) + [xkey], wr=yk)
        st, mv, lr = self.lnst, self.lnmv, self.lnr
        for c in range(2):
            P.op('dve', lambda e, c=c: e.bn_stats(out=st[:, c, :], in_=yt_ap[:, c * 512:(c + 1) * 512]), yk, ["lnst"])
        P.op('dve', lambda e: e.bn_aggr(out=mv[:], in_=st[:].rearrange("p a b -> p (a b)")), ["lnst"], ["lnmv"])
        self.act(lr[:], mv[:, 1:2], AF.Ln, bias=1e-5, rd=["lnmv"], wr=["lnr"])
        self.act(lr[:], lr[:], AF.Exp, scale=-0.5, rd=["lnr"], wr=["lnr"])
        self.ts(yt_ap, yt_ap, mv[:, 0:1], lr[:, 0:1], ALU.subtract, ALU.mult, rd=list(yk) + ["lnmv", "lnr"], wr=yk)
        self.tt(yt_ap, yt_ap, self.lng[:], ALU.mult, rd=list(yk) + ["lng"], wr=yk, eng='pool')
        self.tt(yt_ap, yt_ap, self.lnb[:], ALU.add, rd=list(yk) + ["lnb"], wr=yk, eng='pool')
        P.dma(dst_rows, yt_ap, reads=yk, writes=["dram_store"], key=f"st_y{t % 4}")


_CACHE = {}


def _consts():
    i = np.arange(128)
    c = {}
    c['c_ident'] = np.eye(128, dtype=np.float32)
    c['c_tri'] = (i[:, None] <= i[None, :]).astype(np.float32)
    c['c_ones'] = np.ones((128, 128), np.float32)
    c['c_mneg_sl'] = np.where(i[None, :] < i[:, None], 0.0, NEG).astype(np.float32)
    c['c_mneg_iu'] = np.where(i[:, None] <= i[None, :], 0.0, NEG).astype(np.float32)
    c['c_causal'] = (i[:, None] <= i[None, :]).astype(np.float32)
    sel = np.zeros((32, 32, 128), np.float32)
    for e in range(32):
        sel[e, e, :] = 1.0
    c['c_sel'] = sel.reshape(32, 32 * 128)
    return c


def make_in_maps(inputs, n_cores=N_CORES):
    f = lambda a: np.ascontiguousarray(np.asarray(a, dtype=np.float32))
    shared = {}
    for k in ['w_in', 'gdn_norm_w', 'gla_w_gate2', 'gla_b_gate', 'gla_norm_w',
              'p_gdn', 'p_fox', 'p_gla', 'b_merge', 'w_out', 'ln1_g', 'ln1_b', 'xa_wq', 'xa_wkv', 'xa_wo', 'ln2_g', 'ln2_b',
              'moe_w_gate', 'moe_w_up', 'moe_w_down', 'ln3_g', 'ln3_b']:
        shared[k] = f(inputs[k])
    cw = np.transpose(np.asarray(inputs['gdn_conv_w']), (0, 2, 1))
    shared['conv_wT'] = f(cw.reshape(DEPTH, 12, 128, 4).transpose(0, 2, 1, 3).reshape(DEPTH, 128, 48))
    w_in = np.asarray(inputs['w_in'])
    gcols = list(range(OFF['gdn_b'], OFF['gdn_b'] + 8)) + list(range(OFF['fox_f'], OFF['fox_f'] + 8)) + list(range(OFF['gla_lr'], OFF['gla_lr'] + 16))
    wg_ = w_in[:, :, gcols]
    shared['w_gate_r'] = f(wg_.reshape(DEPTH, 8, 128, 32).transpose(0, 2, 1, 3).reshape(DEPTH, 128, 256))
    wr_ = np.concatenate([np.asarray(inputs['moe_w_group']), np.asarray(inputs['moe_w_expert'])], axis=-1)
    shared['moe_wr_r'] = f(wr_.reshape(DEPTH, 8, 128, 36).transpose(0, 2, 1, 3).reshape(DEPTH, 128, 288))
    shared['alog_r'] = f(np.tile(np.asarray(inputs['gdn_a_log']), (1, NT)))
    shared['dtb_r'] = f(np.tile(np.asarray(inputs['gdn_dt_bias']), (1, NT)))
    shared['fb_r'] = f(np.tile(np.asarray(inputs['fox_f_bias']), (1, NT)))
    shared['moe_br'] = f(np.concatenate([np.asarray(inputs['moe_b_group']), np.asarray(inputs['moe_b_expert'])], axis=-1))
    shared.update(_consts())
    x = f(inputs['x'])
    mem = f(inputs['mem'])
    maps = []
    for c in range(n_cores):
        m = dict(shared)
        m['x'] = x[c * NSEQ:(c + 1) * NSEQ].reshape(NTOK, D)
        m['mem'] = mem[c * NSEQ:(c + 1) * NSEQ].reshape(NSEQ * 256, D)
        maps.append(m)
    return maps


def kernel(**inputs):
    if 'nc' not in _CACHE:
        _CACHE['nc'] = Builder().build()
    nc = _CACHE['nc']
    maps = make_in_maps(inputs)
    res = run_bass_kernel_spmd(nc, maps, core_ids=list(range(N_CORES)))
    out = np.concatenate([r["out"].reshape(NSEQ, S, D) for r in res.results], axis=0)
    return out.astype(np.float32)
```
